# Optimizing a Trainium2 kernel written in Bass

```python
import math
import jax
import jax.numpy as jnp
from jax import lax
import numpy as np

D_MODEL = 1024
BATCH = 8
SEQ = 2048
DEPTH = 2

GRID_W = 64
CTX_LEN = 256

HG_HEADS = 4
HG_DK = 128
HG_DV = 128
RET_HEADS = 4
RET_DK = 64
RET_DV = 128
GQA_HEADS = 4
GQA_KV_HEADS = 2
GQA_HD = 128
DIFF_HEADS = 4
DIFF_HD = 64

N_EXPERTS = 32
TOP_K = 4
D_EXPERT = D_MODEL
SWIGLU_LIMIT = 7.0
SWIGLU_ALPHA = 1.702

CHUNK = 64
Q_BLOCK = 128
MOE_BLOCK = 256
ROPE_THETA = 10000.0
NORM_EPS = 1e-6

N_REC_LAYERS = (DEPTH + 1) // 2
N_ATT_LAYERS = DEPTH // 2

HG_W = HG_HEADS * HG_DK
REC_SPLITS = (HG_W, HG_W, HG_W, HG_HEADS * HG_DV, HG_HEADS * HG_DV,
              RET_HEADS * RET_DK, RET_HEADS * RET_DK, RET_HEADS * RET_DV, RET_HEADS * RET_DV)
REC_IN = sum(REC_SPLITS)
REC_MIX = HG_HEADS * HG_DV + RET_HEADS * RET_DV
ATT_SPLITS = (GQA_HEADS * GQA_HD, GQA_KV_HEADS * GQA_HD, GQA_KV_HEADS * GQA_HD,
              DIFF_HEADS * 2 * DIFF_HD, DIFF_HEADS * 2 * DIFF_HD, DIFF_HEADS * 2 * DIFF_HD)
ATT_IN = sum(ATT_SPLITS)
ATT_MIX = GQA_HEADS * GQA_HD + DIFF_HEADS * 2 * DIFF_HD

kernel_name = 'hybrid_flow_backbone'

F32 = jnp.float32


def rms_norm(x, gain):
    xf = x.astype(F32)
    y = xf * lax.rsqrt(jnp.mean(xf * xf, axis=-1, keepdims=True) + NORM_EPS)
    return (y * gain.astype(F32)).astype(x.dtype)


def head_group_norm(x, gain):
    xf = x.astype(F32)
    xc = xf - jnp.mean(xf, axis=-1, keepdims=True)
    y = xc * lax.rsqrt(jnp.mean(xc * xc, axis=-1, keepdims=True) + NORM_EPS)
    return (y * gain.astype(F32)).astype(x.dtype)


def _split(x, sizes):
    cuts = [int(s) for s in np.cumsum(sizes)[:-1]]
    return jnp.split(x, cuts, axis=-1)


def _heads(x, n_heads):
    b, t, _ = x.shape
    return x.reshape(b, t, n_heads, -1).transpose(0, 2, 1, 3)


def _unheads(x):
    b, h, t, d = x.shape
    return x.transpose(0, 2, 1, 3).reshape(b, t, h * d)


def axial_rope(n_tok, head_dim):
    rows = n_tok // GRID_W
    row = jnp.broadcast_to(jnp.arange(rows, dtype=F32)[:, None], (rows, GRID_W)).reshape(-1)
    col = jnp.broadcast_to(jnp.arange(GRID_W, dtype=F32)[None, :], (rows, GRID_W)).reshape(-1)
    axis_dim = head_dim // 2
    inv_freq = ROPE_THETA ** (-jnp.arange(0, axis_dim, 2, dtype=F32) / axis_dim)
    ang = jnp.concatenate([row[:, None] * inv_freq, col[:, None] * inv_freq], axis=-1)
    return jnp.cos(ang), jnp.sin(ang)


def apply_rope(x, cos, sin):
    xf = x.astype(F32).reshape(*x.shape[:-1], -1, 2)
    x1, x2 = xf[..., 0], xf[..., 1]
    y = jnp.stack([x1 * cos - x2 * sin, x1 * sin + x2 * cos], axis=-1)
    return y.reshape(x.shape).astype(x.dtype)


def modulation(cond, w_ada, b_ada):
    m = jax.nn.silu(cond) @ w_ada + b_ada
    return jnp.split(m[..., None, :], 6, axis=-1)


def chunk_recurrence(q, k, v, log_f, s0):
    bsz, h, t, _ = q.shape
    dv = v.shape[-1]
    n = t // CHUNK

    def rs(a):
        return a.astype(F32).reshape(a.shape[0], a.shape[1], n, CHUNK, a.shape[-1])

    q, k, v, log_f = rs(q), rs(k), rs(v), rs(log_f)
    b = jnp.cumsum(log_f, axis=3)
    b_last = b[:, :, :, -1:, :]
    q_in = q * jnp.exp(b)
    k_in = k * jnp.exp(-b)
    k_state = k * jnp.exp(b_last - b)
    mask = jnp.tril(jnp.ones((CHUNK, CHUNK), dtype=bool))
    att = jnp.where(mask, jnp.einsum('bhncd,bhnsd->bhncs', q_in, k_in), 0.0)
    o_intra = jnp.einsum('bhncs,bhnsv->bhncv', att, v)
    kv = jnp.einsum('bhnsd,bhnsv->nbhdv', k_state, v)
    decay = jnp.moveaxis(jnp.exp(b_last[:, :, :, 0, :]), 2, 0)

    def step(s, inp):
        d, kv_n = inp
        return s * d[..., None] + kv_n, s

    s_last, s_prev = lax.scan(step, s0.astype(F32), (decay, kv))
    o_inter = jnp.einsum('bhncd,nbhdv->bhncv', q_in, s_prev)
    return (o_intra + o_inter).reshape(bsz, h, t, dv), s_last


def _directional(q, k, v, log_f, s0, reverse):
    if reverse:
        fl = lambda a: jnp.flip(a, axis=2)
        o, s = chunk_recurrence(fl(q), fl(k), fl(v), fl(log_f), s0)
        return jnp.flip(o, axis=2), s
    return chunk_recurrence(q, k, v, log_f, s0)


def _to_blocks(x):
    *lead, t, d = x.shape
    return jnp.moveaxis(x.reshape(*lead, t // Q_BLOCK, Q_BLOCK, d), -3, 0)


def _from_blocks(y):
    y = jnp.moveaxis(y, 0, -3)
    *lead, nb, qb, d = y.shape
    return y.reshape(*lead, nb * qb, d)


def gqa_attend(q, k, v):
    scale = q.shape[-1] ** -0.5

    def block(qb):
        s = jnp.einsum('bkgqd,bksd->bkgqs', qb, k).astype(F32) * scale
        p = jax.nn.softmax(s, axis=-1).astype(v.dtype)
        return jnp.einsum('bkgqs,bksd->bkgqd', p, v)

    return _from_blocks(lax.map(block, _to_blocks(q)))


def diff_attend(q, k, v, lam):
    scale = q.shape[-1] ** -0.5

    def block(qb):
        s = jnp.einsum('bhiqd,bhisd->bhiqs', qb, k).astype(F32) * scale
        p = jax.nn.softmax(s, axis=-1)
        a = (p[:, :, 0] - lam * p[:, :, 1]).astype(v.dtype)
        return jnp.einsum('bhqs,bhsv->bhqv', a, v)

    return _from_blocks(lax.map(block, _to_blocks(q)))


def recurrent_mixer(a_lat, a_ctx, w_in, lb, w_out, hg_gain, ret_gain, rope_r, need_ctx):
    log_gamma = jnp.log(1.0 - 2.0 ** (-5.0 - jnp.arange(RET_HEADS, dtype=F32)))
    log_gammas = (log_gamma, log_gamma[::-1])

    def prep(a, rope):
        hq, hf_f, hf_b, hi, hg, rq, rk, rv, rg = _split(a @ w_in, REC_SPLITS)
        q = jax.nn.silu(_heads(hq, HG_HEADS).astype(F32)) * HG_DK ** -0.5
        f = tuple(_heads(lb[d] + (1.0 - lb[d]) * jax.nn.sigmoid(z.astype(F32)), HG_HEADS)
                  for d, z in enumerate((hf_f, hf_b)))
        i = _heads(hi, HG_HEADS)
        rq_h = _heads(rq, RET_HEADS)
        rk_h = _heads(rk, RET_HEADS) * RET_DK ** -0.5
        if rope is not None:
            rq_h, rk_h = apply_rope(rq_h, *rope), apply_rope(rk_h, *rope)
        return q, f, i, hg, rq_h, rk_h, _heads(rv, RET_HEADS), rg

    cq, cf, ci, cg, crq, crk, crv, crg = prep(a_ctx, None)
    lq, lf, li, lg, lrq, lrk, lrv, lrg = prep(a_lat, rope_r)
    bsz, n_ctx, n_lat = a_lat.shape[0], a_ctx.shape[1], a_lat.shape[1]
    hg_c = hg_l = ret_c = ret_l = 0.0
    for d, rev in enumerate((False, True)):
        s0 = jnp.zeros((bsz, HG_HEADS, HG_DK, HG_DV), F32)
        o, s = _directional(cq, 1.0 - cf[d], ci, jnp.log(cf[d]), s0, rev)
        if need_ctx:
            hg_c = hg_c + o
        o, _ = _directional(lq, 1.0 - lf[d], li, jnp.log(lf[d]), s, rev)
        hg_l = hg_l + o
        s0 = jnp.zeros((bsz, RET_HEADS, RET_DK, RET_DV), F32)
        dec = log_gammas[d][None, :, None, None]
        o, s = _directional(crq, crk, crv, jnp.broadcast_to(dec, (1, RET_HEADS, n_ctx, 1)), s0, rev)
        if need_ctx:
            ret_c = ret_c + o
        o, _ = _directional(lrq, lrk, lrv, jnp.broadcast_to(dec, (1, RET_HEADS, n_lat, 1)), s, rev)
        ret_l = ret_l + o

    def merge(o_hg, o_ret, g_hg, g_ret, dtype):
        y_hg = rms_norm(o_hg, hg_gain) * jax.nn.silu(_heads(g_hg, HG_HEADS).astype(F32))
        y_ret = head_group_norm(o_ret, ret_gain) * jax.nn.silu(_heads(g_ret, RET_HEADS).astype(F32))
        y = jnp.concatenate([_unheads(y_hg), _unheads(y_ret)], axis=-1).astype(dtype)
        return y @ w_out

    y_lat = merge(hg_l, ret_l, lg, lrg, a_lat.dtype)
    y_ctx = merge(hg_c, ret_c, cg, crg, a_ctx.dtype) if need_ctx else None
    return y_lat, y_ctx


def attention_mixer(a_lat, a_ctx, w_in, w_out, q_gain, k_gain, dq_gain, dk_gain, lam_params,
                    diff_gain, lam_init, rope_g, rope_d, need_ctx):
    lp = lam_params.astype(F32)
    lam = jnp.exp(jnp.sum(lp[0] * lp[1])) - jnp.exp(jnp.sum(lp[2] * lp[3])) + lam_init

    def prep(a, ropes):
        gq, gk, gv, dq, dk, dv = _split(a @ w_in, ATT_SPLITS)
        b, t, _ = a.shape
        gq = rms_norm(_heads(gq, GQA_HEADS), q_gain)
        gk = rms_norm(_heads(gk, GQA_KV_HEADS), k_gain)
        dq = rms_norm(_heads(dq, 2 * DIFF_HEADS), dq_gain)
        dk = rms_norm(_heads(dk, 2 * DIFF_HEADS), dk_gain)
        if ropes is not None:
            (cg, sg), (cd, sd) = ropes
            gq, gk = apply_rope(gq, cg, sg), apply_rope(gk, cg, sg)
            dq, dk = apply_rope(dq, cd, sd), apply_rope(dk, cd, sd)
        gq = gq.reshape(b, GQA_KV_HEADS, GQA_HEADS // GQA_KV_HEADS, t, GQA_HD)
        dq = dq.reshape(b, DIFF_HEADS, 2, t, DIFF_HD)
        dk = dk.reshape(b, DIFF_HEADS, 2, t, DIFF_HD)
        return gq, gk, _heads(gv, GQA_KV_HEADS), dq, dk, _heads(dv, DIFF_HEADS)

    cgq, cgk, cgv, cdq, cdk, cdv = prep(a_ctx, None)
    lgq, lgk, lgv, ldq, ldk, ldv = prep(a_lat, (rope_g, rope_d))
    cat = lambda u, w: jnp.concatenate([u, w], axis=-2)

    def merge(o_g, o_d, dtype):
        b, _, _, t, _ = o_g.shape
        o_g = o_g.reshape(b, GQA_HEADS, t, GQA_HD)
        o_d = rms_norm(o_d, diff_gain) * (1.0 - lam_init)
        y = jnp.concatenate([_unheads(o_g), _unheads(o_d)], axis=-1).astype(dtype)
        return y @ w_out

    y_lat = merge(gqa_attend(lgq, cat(cgk, lgk), cat(cgv, lgv)),
                  diff_attend(ldq, cat(cdk, ldk), cat(cdv, ldv), lam), a_lat.dtype)
    y_ctx = merge(gqa_attend(cgq, cgk, cgv), diff_attend(cdq, cdk, cdv, lam), a_ctx.dtype) if need_ctx else None
    return y_lat, y_ctx


def clamped_swiglu(gu):
    gate, up = jnp.split(gu, 2, axis=-1)
    gate = jnp.minimum(gate, SWIGLU_LIMIT)
    up = jnp.clip(up, -SWIGLU_LIMIT, SWIGLU_LIMIT)
    return (up + 1.0) * gate * jax.nn.sigmoid(SWIGLU_ALPHA * gate)


def moe(xf, w_router, b_router, w_gu, b_gu, w_down, b_down):
    n, d = xf.shape
    logits = (xf @ w_router + b_router).astype(F32)
    top_val, top_idx = lax.top_k(logits, TOP_K)
    gates = jax.nn.softmax(top_val, axis=-1).reshape(-1)
    flat_e = top_idx.reshape(-1)
    n_assign = n * TOP_K
    order = jnp.argsort(flat_e)
    sorted_e = flat_e[order]
    tok = order // TOP_K
    counts = jnp.zeros((N_EXPERTS,), jnp.int32).at[flat_e].add(1)
    padded = (counts + MOE_BLOCK - 1) // MOE_BLOCK * MOE_BLOCK
    pad_end = jnp.cumsum(padded)
    pad_start = pad_end - padded
    start = jnp.cumsum(counts) - counts
    dest = pad_start[sorted_e] + jnp.arange(n_assign, dtype=jnp.int32) - start[sorted_e]
    n_blocks = -(-n_assign // MOE_BLOCK) + N_EXPERTS
    row_tok = jnp.full((n_blocks * MOE_BLOCK,), n, jnp.int32).at[dest].set(tok)
    block_e = jnp.minimum(jnp.searchsorted(pad_end, jnp.arange(n_blocks, dtype=jnp.int32) * MOE_BLOCK,
                                           side='right'), N_EXPERTS - 1)
    x_rows = jnp.concatenate([xf, jnp.zeros((1, d), xf.dtype)], axis=0)[row_tok]
    x_rows = x_rows.reshape(n_blocks, MOE_BLOCK, d)

    def expert_block(args):
        xb, e = args
        hb = clamped_swiglu(xb @ w_gu[e] + b_gu[e])
        return hb @ w_down[e] + b_down[e]

    y_rows = lax.map(expert_block, (x_rows, block_e)).reshape(n_blocks * MOE_BLOCK, d)
    y_assign = y_rows[dest] * gates[order][:, None].astype(y_rows.dtype)
    return jax.ops.segment_sum(y_assign, tok, num_segments=n)


def setup_inputs(seed: int = 0) -> dict:
    key = jax.random.key(seed)
    k = jax.random.split(key, 27)
    nrm = lambda i, shape, scale: jax.random.normal(k[i], shape, F32) * scale
    gain = lambda i, shape: 1.0 + 0.05 * jax.random.normal(k[i], shape, F32)
    D = D_MODEL
    return {
        'x': nrm(0, (BATCH, SEQ, D), 1.0),
        'c': nrm(1, (BATCH, D), 1.0),
        'ctx': nrm(2, (BATCH, CTX_LEN, D), 1.0),
        'c_ctx': nrm(3, (D,), 1.0),
        'norm_mix': gain(4, (DEPTH, D)),
        'norm_ffn': gain(5, (DEPTH, D)),
        'w_ada': nrm(6, (DEPTH, D, 6 * D), 0.5 * D ** -0.5),
        'b_ada': nrm(7, (DEPTH, 6 * D), 0.01),
        'rec_w_in': nrm(8, (N_REC_LAYERS, D, REC_IN), D ** -0.5),
        'rec_lb_logits': nrm(9, (DEPTH + 1, 2, HG_W), 0.5),
        'rec_w_out': nrm(10, (N_REC_LAYERS, REC_MIX, D), REC_MIX ** -0.5),
        'rec_hg_gain': gain(11, (N_REC_LAYERS, HG_DV)),
        'rec_ret_gain': gain(12, (N_REC_LAYERS, RET_DV)),
        'att_w_in': nrm(13, (N_ATT_LAYERS, D, ATT_IN), D ** -0.5),
        'att_w_out': nrm(14, (N_ATT_LAYERS, ATT_MIX, D), ATT_MIX ** -0.5),
        'att_q_gain': gain(15, (N_ATT_LAYERS, GQA_HD)),
        'att_k_gain': gain(16, (N_ATT_LAYERS, GQA_HD)),
        'diff_q_gain': gain(17, (N_ATT_LAYERS, DIFF_HD)),
        'diff_k_gain': gain(18, (N_ATT_LAYERS, DIFF_HD)),
        'diff_lambda': nrm(19, (N_ATT_LAYERS, 4, DIFF_HD), 0.1),
        'diff_gain': gain(20, (N_ATT_LAYERS, 2 * DIFF_HD)),
        'w_router': nrm(21, (DEPTH, D, N_EXPERTS), D ** -0.5),
        'b_router': nrm(22, (DEPTH, N_EXPERTS), 0.01),
        'w_gu': nrm(23, (DEPTH, N_EXPERTS, D, 2 * D_EXPERT), D ** -0.5),
        'b_gu': nrm(24, (DEPTH, N_EXPERTS, 2 * D_EXPERT), 0.01),
        'w_down': nrm(25, (DEPTH, N_EXPERTS, D_EXPERT, D), D_EXPERT ** -0.5),
        'b_down': nrm(26, (DEPTH, N_EXPERTS, D), 0.01),
    }


def reference(x, c, ctx, c_ctx, norm_mix, norm_ffn, w_ada, b_ada, rec_w_in, rec_lb_logits, rec_w_out,
              rec_hg_gain, rec_ret_gain, att_w_in, att_w_out, att_q_gain, att_k_gain, diff_q_gain,
              diff_k_gain, diff_lambda, diff_gain, w_router, b_router, w_gu, b_gu, w_down, b_down):
    d = x.shape[-1]
    n_lat = x.shape[1]
    ropes = {hd: axial_rope(n_lat, hd) for hd in sorted({GQA_HD, DIFF_HD, RET_DK})}
    lb_all = jnp.cumsum(jax.nn.softmax(rec_lb_logits.astype(F32), axis=0), axis=0)
    h_lat, h_ctx = x, ctx
    for l in range(DEPTH):
        need_ctx = l < DEPTH - 1
        sh1, sc1, g1, sh2, sc2, g2 = modulation(c, w_ada[l], b_ada[l])
        csh1, csc1, cg1, csh2, csc2, cg2 = modulation(c_ctx, w_ada[l], b_ada[l])
        a_lat = rms_norm(h_lat, norm_mix[l]) * (1.0 + sc1) + sh1
        a_ctx = rms_norm(h_ctx, norm_mix[l]) * (1.0 + csc1) + csh1
        j = l // 2
        if l % 2 == 0:
            m_lat, m_ctx = recurrent_mixer(a_lat, a_ctx, rec_w_in[j], lb_all[l], rec_w_out[j],
                                           rec_hg_gain[j], rec_ret_gain[j], ropes[RET_DK], need_ctx)
        else:
            m_lat, m_ctx = attention_mixer(a_lat, a_ctx, att_w_in[j], att_w_out[j], att_q_gain[j],
                                           att_k_gain[j], diff_q_gain[j], diff_k_gain[j], diff_lambda[j],
                                           diff_gain[j], 0.8 - 0.6 * math.exp(-0.3 * l),
                                           ropes[GQA_HD], ropes[DIFF_HD], need_ctx)
        h_lat = h_lat + g1 * m_lat
        f_lat = rms_norm(h_lat, norm_ffn[l]) * (1.0 + sc2) + sh2
        moe_args = (w_router[l], b_router[l], w_gu[l], b_gu[l], w_down[l], b_down[l])
        if need_ctx:
            h_ctx = h_ctx + cg1 * m_ctx
            f_ctx = rms_norm(h_ctx, norm_ffn[l]) * (1.0 + csc2) + csh2
            n_l = f_lat.shape[0] * f_lat.shape[1]
            y = moe(jnp.concatenate([f_lat.reshape(-1, d), f_ctx.reshape(-1, d)], axis=0), *moe_args)
            h_lat = h_lat + g2 * y[:n_l].reshape(f_lat.shape)
            h_ctx = h_ctx + cg2 * y[n_l:].reshape(f_ctx.shape)
        else:
            h_lat = h_lat + g2 * moe(f_lat.reshape(-1, d), *moe_args).reshape(f_lat.shape)
    return h_lat
```

```python
import contextlib
import math
import numpy as np
import concourse.bass as bass
import concourse.mybir as mybir
from concourse.bass_utils import run_bass_kernel_spmd

F32 = mybir.dt.float32
BF16 = mybir.dt.bfloat16
AF = mybir.ActivationFunctionType
ALU = mybir.AluOpType
AX = mybir.AxisListType


class Res:
    __slots__ = ("name", "w", "r")

    def __init__(self, name=""):
        self.name = name
        self.w = None
        self.r = []


class _Op:
    __slots__ = ("eng", "fn", "deps", "idx", "sig", "dma", "sem", "val", "nsig")


class Prog:
    COMPUTE = ("pe", "act", "dve", "pool")
    NDMASEM = 8

    def __init__(self):
        self.ops = []
        self.dma_count = {"sp": 0, "act": 0, "pool": 0}
        self.last = {}
        self.dma_recent = {"sp": [], "act": [], "pool": []}

    def add(self, eng, fn, reads=(), writes=(), dma=False, extra_deps=()):
        op = _Op()
        op.eng, op.fn, op.dma = eng, fn, dma
        op.idx = len(self.ops)
        op.sig = None
        op.nsig = False
        deps = set(extra_deps)
        for r in reads:
            if r.w is not None:
                deps.add(r.w)
        for w in writes:
            if w.w is not None:
                deps.add(w.w)
            deps.update(w.r)
        for r in reads:
            r.r.append(op.idx)
        for w in writes:
            w.w = op.idx
            w.r = []
        if dma:
            j = self.dma_count[eng]
            self.dma_count[eng] = j + 1
            op.sem = (eng, j % self.NDMASEM)
            op.val = 16 * (j // self.NDMASEM + 1)
            rec = self.dma_recent[eng]
            rec.append(op.idx)
            if len(rec) > self.NDMASEM:
                rec.pop(0)
        elif fn is not None:
            self.last[eng] = op.idx
        if eng == "pe" and not dma:
            deps = {d for d in deps if not (self.ops[d].eng == "pe" and not self.ops[d].dma)}
        op.deps = deps
        self.ops.append(op)
        return op.idx

    def pe(self, fn, reads=(), writes=()):
        return self.add("pe", fn, reads, writes)

    def act(self, fn, reads=(), writes=()):
        return self.add("act", fn, reads, writes)

    def dve(self, fn, reads=(), writes=()):
        return self.add("dve", fn, reads, writes)

    def pool(self, fn, reads=(), writes=()):
        return self.add("pool", fn, reads, writes)

    def dma(self, q, fn, reads=(), writes=()):
        return self.add(q, fn, reads, writes, dma=True)

    def barrier(self):
        deps = set(self.last.values())
        for q in self.dma_recent:
            deps.update(self.dma_recent[q])
        for e in ("pe", "act", "dve", "pool", "sp"):
            self.add(e, None, extra_deps=deps)

    def emit(self, nc, final_wait_ops=()):
        ops = self.ops
        for op in ops:
            for d in op.deps:
                if not ops[d].dma:
                    ops[d].nsig = True
        for i in final_wait_ops:
            if not ops[i].dma:
                ops[i].nsig = True
        cnt = {e: 0 for e in self.COMPUTE}
        for op in ops:
            if not op.dma and op.nsig:
                cnt[op.eng] += 1
                op.sig = cnt[op.eng]
        engobj = {"pe": nc.tensor, "act": nc.scalar, "dve": nc.vector, "pool": nc.gpsimd, "sp": nc.sync}
        with contextlib.ExitStack() as st:
            esem = {e: st.enter_context(nc.semaphore("s_" + e)) for e in self.COMPUTE}
            dsem = {}
            for q in ("sp", "act", "pool"):
                if self.dma_count[q]:
                    for k in range(self.NDMASEM):
                        dsem[(q, k)] = st.enter_context(nc.semaphore("d_%s%d" % (q, k)))
            block = st.enter_context(nc.Block())
            per_eng = {e: [] for e in engobj}
            for op in ops:
                per_eng[op.eng].append(op)

            def gen(ename):
                eng = engobj[ename]
                waited = {}

                def need(key, semh, val):
                    if waited.get(key, 0) >= val:
                        return
                    waited[key] = val
                    eng.wait_ge(semh, val)

                for op in per_eng[ename]:
                    for d in sorted(op.deps):
                        dop = ops[d]
                        if dop.dma:
                            need(dop.sem, dsem[dop.sem], dop.val)
                        else:
                            need(dop.eng, esem[dop.eng], dop.sig)
                    if op.fn is None:
                        continue
                    if op.dma and op.val > 16:
                        need(op.sem, dsem[op.sem], op.val - 16)
                    ins = op.fn(eng)
                    if op.dma:
                        ins.then_inc(dsem[op.sem], 16)
                    elif op.sig is not None:
                        ins.then_inc(esem[op.eng], 1)
                if ename == "sp":
                    for i in final_wait_ops:
                        fop = ops[i]
                        if fop.dma:
                            need(fop.sem, dsem[fop.sem], fop.val)
                        else:
                            need(fop.eng, esem[fop.eng], fop.sig)

            @block.sync
            def _(e):
                gen("sp")

            @block.tensor
            def _(e):
                gen("pe")

            @block.scalar
            def _(e):
                gen("act")

            @block.vector
            def _(e):
                gen("dve")

            @block.gpsimd
            def _(e):
                gen("pool")


D = 1024
KC = 8
NCTX = 256
NLAT = 2048
T = NCTX + NLAT
NE = 32
DEPTH = 2
CH = [(0, 256), (256, 512), (768, 512), (1280, 512), (1792, 512)]
EPS = 1e-6
LIMIT = 7.0
ALPHA = 1.702

V_BADA = 0
V_NMIX = V_BADA + DEPTH * 48
V_NFFN = V_NMIX + DEPTH * 8
V_BGU = V_NFFN + DEPTH * 8
V_ATT = V_BGU + DEPTH * NE * 16
V_REC = V_ATT + 9
NVEC = V_REC + 26


_UNIQ = [0]


def _sb(nc, name, shape, dtype):
    _UNIQ[0] += 1
    return nc.sbuf_tensor("%s_u%d" % (name, _UNIQ[0]), shape, dtype)


class K:
    pass


def mm(P, out, lhsT, rhs, start, stop, reads, writes):
    return P.pe(lambda e: e.matmul(out, lhsT=lhsT, rhs=rhs, start=start, stop=stop), reads, writes)


def tr(P, out, in_, ident, reads, writes):
    return P.pe(lambda e: e.transpose(out, in_, ident), reads, writes)


def act(P, out, in_, func, reads, writes, bias=0.0, scale=1.0, accum_out=None, eng="act"):
    if accum_out is None:
        return P.add(eng, lambda e: e.activation(out=out, in_=in_, func=func, bias=bias, scale=scale), reads, writes)
    return P.add(eng, lambda e: e.activation(out=out, in_=in_, func=func, bias=bias, scale=scale, accum_out=accum_out), reads, writes)


def ts(P, eng, out, in0, s1, s2, op0, op1, reads, writes, accum_out=None):
    if op1 is None:
        return P.add(eng, lambda e: e.tensor_scalar(out=out, in0=in0, scalar1=s1, scalar2=None, op0=op0), reads, writes)
    if accum_out is not None:
        return P.add(eng, lambda e: e.tensor_scalar(out=out, in0=in0, scalar1=s1, scalar2=s2, op0=op0, op1=op1, accum_out=accum_out), reads, writes)
    return P.add(eng, lambda e: e.tensor_scalar(out=out, in0=in0, scalar1=s1, scalar2=s2, op0=op0, op1=op1), reads, writes)


def tt(P, eng, out, in0, in1, op, reads, writes):
    return P.add(eng, lambda e: e.tensor_tensor(out=out, in0=in0, in1=in1, op=op), reads, writes)


def stt(P, out, in0, scalar, in1, op0, op1, reads, writes, accum_out=None):
    if accum_out is None:
        return P.dve(lambda e: e.scalar_tensor_tensor(out=out, in0=in0, scalar=scalar, in1=in1, op0=op0, op1=op1), reads, writes)
    return P.dve(lambda e: e.scalar_tensor_tensor(out=out, in0=in0, scalar=scalar, in1=in1, op0=op0, op1=op1, accum_out=accum_out), reads, writes)


def cp(P, eng, out, in_, reads, writes):
    return P.add(eng, lambda e: e.tensor_copy(out=out, in_=in_), reads, writes)


def dma(P, q, out, in_, reads, writes):
    return P.dma(q, lambda e: e.dma_start(out=out, in_=in_), reads, writes)


def getps(k):
    i = k.ps_next % k.ps_n
    k.ps_next = (i + 1) % k.ps_n
    return k.ps[i], k.psr[i]


def load_tokens(k):
    P, nc = k.P, k.nc
    with contextlib.ExitStack() as st:
        stage = [st.enter_context(_sb(nc, "ldst%d" % i, [128, D], F32)) for i in range(3)]
        sres = [Res() for _ in range(3)]
        for ti in range(T // 128):
            s = ti % 3
            src = k.ctx_d[ti * 128:(ti + 1) * 128, :] if ti < 2 else k.x_d[(ti - 2) * 128:(ti - 1) * 128, :]
            dma(P, "sp", stage[s][:], src, [], [sres[s]])
            ci, off = chunk_of(ti * 128)
            for half in range(2):
                ps, pr = getps(k)
                for j in range(4):
                    kc = half * 4 + j
                    tr(P, ps[:, j * 128:(j + 1) * 128], stage[s][:, kc * 128:(kc + 1) * 128], k.identf[:], [sres[s], k.cres], [pr])
                eng = "dve" if half == 0 else "act"
                dst = k.hT[:, half * 4:half * 4 + 4, ti * 128:(ti + 1) * 128]
                src_ps = ps[:, :].rearrange("p (j t) -> p j t", j=4)
                wr = [k.hres[half * 4 + j][ci] for j in range(4)]
                if eng == "dve":
                    cp(P, "dve", dst, src_ps, [pr], wr)
                else:
                    act(P, dst, src_ps, AF.Copy, [pr], wr)
        P.barrier()


def chunk_of(t):
    for ci, (t0, tn) in enumerate(CH):
        if t0 <= t < t0 + tn:
            return ci, t - t0
    raise ValueError


def store_output(k):
    P, nc = k.P, k.nc
    outs = []
    with contextlib.ExitStack() as st:
        stage = [st.enter_context(_sb(nc, "stst%d" % i, [128, D], F32)) for i in range(3)]
        sres = [Res() for _ in range(3)]
        for ti in range(2, T // 128):
            s = ti % 3
            ci, off = chunk_of(ti * 128)
            for half in range(2):
                ps, pr = getps(k)
                for j in range(4):
                    kc = half * 4 + j
                    tr(P, ps[:, j * 128:(j + 1) * 128], k.hT[:, kc, ti * 128:(ti + 1) * 128], k.identf[:], [k.hres[kc][ci], k.cres], [pr])
                dst = stage[s][:, half * 512:(half + 1) * 512]
                if half == 0:
                    cp(P, "dve", dst, ps[:, :], [pr], [sres[s]])
                else:
                    act(P, dst, ps[:, :], AF.Copy, [pr], [sres[s]])
            outs.append(dma(P, "sp", k.out_d[(ti - 2) * 128:(ti - 1) * 128, :], stage[s][:], [sres[s]], []))
        P.barrier()
    return outs


def modulation(k):
    P, nc = k.P, k.nc
    with contextlib.ExitStack() as st:
        ccf = st.enter_context(_sb(nc, "ccf", [128, KC, 2], F32))
        sT = st.enter_context(_sb(nc, "sT", [128, KC, 2], BF16))
        wt = [st.enter_context(_sb(nc, "wada%d" % i, [128, KC, 768], BF16)) for i in range(2)]
        wres = [Res() for _ in range(2)]
        rcc, rs = Res(), Res()
        dma(P, "sp", ccf[:], k.cc_d[:, :, :], [], [rcc])
        act(P, sT[:], ccf[:], AF.Silu, [rcc], [rs])
        n = 0
        for l in range(DEPTH):
            ps, pr = getps(k)
            for g in range(8):
                s = n % 2
                n += 1
                src = k.wada_d[l].rearrange("(kc p) f -> p kc f", p=128)[:, :, g * 768:(g + 1) * 768]
                dma(P, "pool", wt[s][:], src, [], [wres[s]])
                for jj in range(6):
                    j = g * 6 + jj
                    for kc in range(KC):
                        mm(P, ps[:, 2 * j:2 * j + 2], wt[s][:, kc, jj * 128:(jj + 1) * 128], sT[:, kc, :],
                           kc == 0, kc == KC - 1, [wres[s], rs], [pr])
            for w in range(2):
                src_ps = ps[:, 0:96].rearrange("p (j w) -> p j w", w=2)[:, :, w]
                tt(P, "dve", k.mod[l][:, w, :], src_ps, k.vecs[:, V_BADA + l * 48:V_BADA + (l + 1) * 48], ALU.add,
                   [pr, k.cres], [k.modres])
                stt(P, k.A1[l][:, w, :], k.mod[l][:, w, 8:16], 1.0, k.vecs[:, V_NMIX + l * 8:V_NMIX + l * 8 + 8], ALU.add, ALU.mult,
                    [k.modres, k.cres], [k.modres])
                stt(P, k.A2[l][:, w, :], k.mod[l][:, w, 32:40], 1.0, k.vecs[:, V_NFFN + l * 8:V_NFFN + l * 8 + 8], ALU.add, ALU.mult,
                    [k.modres, k.cres], [k.modres])
        P.barrier()


def norm_mod(k, l, which, f32cb=None, skip_ctx=False):
    P, nc = k.P, k.nc
    A = k.A1[l] if which == 0 else k.A2[l]
    shj = 0 if which == 0 else 24
    with contextlib.ExitStack() as st:
        sq = [st.enter_context(_sb(nc, "nsq%d" % i, [128, 512], F32)) for i in range(2)]
        sqr = [Res() for _ in range(2)]
        rstd = [st.enter_context(_sb(nc, "nrs%d" % i, [128, 512], F32)) for i in range(2)]
        rsr = [Res() for _ in range(2)]
        tmp = [st.enter_context(_sb(nc, "ntm%d" % i, [128, 512], F32)) for i in range(2)]
        tmr = [Res() for _ in range(2)]
        if f32cb is not None:
            f32t = [st.enter_context(_sb(nc, "nf32%d" % i, [128, KC, 512], F32)) for i in range(2)]
            f32r = [Res() for _ in range(2)]
        cnt = {"nsq": 0, "ntm": 0}
        pss = {}

        def stage1(ci):
            t0, tn = CH[ci]
            ps, pr = getps(k)
            for kc in range(KC):
                s = cnt["nsq"] % 2
                cnt["nsq"] += 1
                act(P, sq[s][:, :tn], k.hT[:, kc, t0:t0 + tn], AF.Square, [k.hres[kc][ci]], [sqr[s]])
                mm(P, ps[:, :tn], k.onesf[:], sq[s][:, :tn], kc == 0, kc == KC - 1, [sqr[s], k.cres], [pr])
            r = ci % 2
            act(P, rstd[r][:, :tn], ps[:, :tn], AF.Ln, [pr], [rsr[r]], bias=k.epsc[:, 0:1], scale=1.0 / D)
            act(P, rstd[r][:, :tn], rstd[r][:, :tn], AF.Exp, [rsr[r]], [rsr[r]], scale=-0.5)

        def stage2(ci):
            t0, tn = CH[ci]
            w = 1 if ci == 0 else 0
            r = ci % 2
            for kc in range(KC):
                s = cnt["ntm"] % 2
                cnt["ntm"] += 1
                stt(P, tmp[s][:, :tn], k.hT[:, kc, t0:t0 + tn], A[:, w, kc:kc + 1], rstd[r][:, :tn], ALU.mult, ALU.mult,
                    [k.hres[kc][ci], k.modres, rsr[r]], [tmr[s]])
                shc = k.mod[l][:, w, shj + kc:shj + kc + 1]
                if f32cb is None:
                    act(P, k.xT[:, kc, t0:t0 + tn], tmp[s][:, :tn], AF.Identity, [tmr[s], k.modres], [k.xres[kc][ci]], bias=shc)
                else:
                    fr = ci % 2
                    act(P, f32t[fr][:, kc, :tn], tmp[s][:, :tn], AF.Identity, [tmr[s], k.modres], [f32r[fr]], bias=shc)
                    cp(P, "pool", k.xT[:, kc, t0:t0 + tn], f32t[fr][:, kc, :tn], [f32r[fr]], [k.xres[kc][ci]])
            if f32cb is not None:
                f32cb(ci, t0, tn, f32t[ci % 2], f32r[ci % 2])

        cis = [ci for ci in range(len(CH)) if not (skip_ctx and ci == 0)]
        stage1(cis[0])
        for i_, ci in enumerate(cis):
            if i_ + 1 < len(cis):
                stage1(cis[i_ + 1])
            stage2(ci)
        P.barrier()


def moe_layer(k, l):
    P, nc = k.P, k.nc
    with contextlib.ExitStack() as st:
        last = (l == DEPTH - 1)
        GT = st.enter_context(_sb(nc, "GT", [NE, T], F32))
        gtres = [Res() for _ in CH]

        with contextlib.ExitStack() as st2:
            wr_t = st2.enter_context(_sb(nc, "wrt", [128, KC, NE], F32))
            br_t = st2.enter_context(_sb(nc, "brt", [1, NE], F32))
            bd_t = st2.enter_context(_sb(nc, "bdt", [NE, D], F32))
            wres = Res()
            dma(P, "sp", wr_t[:], k.wrouter_d[l].rearrange("(kc p) e -> p kc e", p=128), [], [wres])
            dma(P, "sp", br_t[:], k.brouter_d[l:l + 1, :], [], [wres])
            dma(P, "sp", bd_t[:], k.bdown_d[l], [], [wres])
            lg = [st2.enter_context(_sb(nc, "lg%d" % i, [128, NE], F32)) for i in range(2)]
            ex = [st2.enter_context(_sb(nc, "ex%d" % i, [128, NE], F32)) for i in range(2)]
            msk = [st2.enter_context(_sb(nc, "mk%d" % i, [128, NE], F32)) for i in range(2)]
            t8 = [st2.enter_context(_sb(nc, "t8%d" % i, [128, 8], F32)) for i in range(2)]
            den = [st2.enter_context(_sb(nc, "dn%d" % i, [128, 2], F32)) for i in range(2)]
            rr = [Res() for _ in range(2)]
            cnt = [0]

            def router_cb(ci, t0, tn, f32tile, f32res):
                for sub in range(tn // 128):
                    s = cnt[0] % 2
                    cnt[0] += 1
                    ps, pr = getps(k)
                    for kc in range(KC):
                        mm(P, ps[:, 0:NE], f32tile[:, kc, sub * 128:(sub + 1) * 128], wr_t[:, kc, :], kc == 0, False,
                           [f32res, wres], [pr])
                    mm(P, ps[:, 0:NE], k.onesf[0:1, :], br_t[0:1, :], False, True, [wres, k.cres], [pr])
                    cp(P, "dve", lg[s][:], ps[:, 0:NE], [pr], [rr[s]])
                    P.dve(lambda e, o=t8[s][:], i=lg[s][:]: e.max(out=o, in_=i), [rr[s]], [rr[s]])
                    ts(P, "dve", msk[s][:], lg[s][:], t8[s][:, 3:4], None, ALU.is_ge, None, [rr[s]], [rr[s]])
                    ts(P, "dve", den[s][:, 1:2], t8[s][:, 0:1], -1.0, None, ALU.mult, None, [rr[s]], [rr[s]])
                    act(P, ex[s][:], lg[s][:], AF.Exp, [rr[s]], [rr[s]], bias=den[s][:, 1:2])
                    stt(P, ex[s][:], ex[s][:], 1.0, msk[s][:], ALU.mult, ALU.mult, [rr[s]], [rr[s]], accum_out=den[s][:, 0:1])
                    P.dve(lambda e, o=den[s][:, 0:1]: e.reciprocal(out=o, in_=o), [rr[s]], [rr[s]])
                    ts(P, "dve", ex[s][:], ex[s][:], den[s][:, 0:1], None, ALU.mult, None, [rr[s]], [rr[s]])
                    ps2, pr2 = getps(k)
                    tr(P, ps2[0:NE, 0:128], ex[s][:], k.identf[:], [rr[s], k.cres], [pr2])
                    cp(P, "dve", GT[:, t0 + sub * 128:t0 + (sub + 1) * 128], ps2[0:NE, 0:128], [pr2], [gtres[ci]])

            norm_mod(k, l, 1, f32cb=router_cb, skip_ctx=last)

            for ci, (t0, tn) in enumerate(CH):
                if last and ci == 0:
                    continue
                w = 1 if ci == 0 else 0
                for dc in range(KC):
                    ps, pr = getps(k)
                    mm(P, ps[:, :tn], bd_t[:, dc * 128:(dc + 1) * 128], GT[:, t0:t0 + tn], True, True, [wres, gtres[ci]], [pr])
                    stt(P, k.hT[:, dc, t0:t0 + tn], ps[:, :tn], k.mod[l][:, w, 40 + dc:41 + dc], k.hT[:, dc, t0:t0 + tn], ALU.mult, ALU.add,
                        [pr, k.modres, k.hres[dc][ci]], [k.hres[dc][ci]])
            P.barrier()

        NSLOT = 8
        PW = 256
        GW = 768
        ring = [st.enter_context(_sb(nc, "wring%d" % i, [128, KC, PW], BF16)) for i in range(NSLOT)]
        ringres = [Res() for _ in range(NSLOT)]
        if last:
            groups = [[(256, 512, 1), (768, 256, 2)], [(1024, 256, 2), (1280, 512, 3)], [(1792, 512, 4)]]
        else:
            groups = [[(0, 256, 0), (256, 512, 1)], [(768, 512, 2), (1280, 256, 3)], [(1536, 256, 3), (1792, 512, 4)]]
        hm = [st.enter_context(_sb(nc, "hmid%d" % i, [128, KC, GW], BF16)) for i in range(2)]
        hmres = [[[Res() for _ in range(2)] for _ in range(KC)] for _ in range(2)]
        gb = [st.enter_context(_sb(nc, "gb%d" % i, [128, GW], F32)) for i in range(2)]
        gbres = [[Res() for _ in range(2)] for _ in range(2)]
        NT = 4
        SKEW = 1
        gc = [st.enter_context(_sb(nc, "gc%d" % i, [128, 512], F32)) for i in range(NT)]
        u0 = [st.enter_context(_sb(nc, "u0%d" % i, [128, 512], F32)) for i in range(NT)]
        tres = [Res() for _ in range(NT)]

        items = [(gi, e) for gi in range(len(groups)) for e in range(NE)]
        pieces = []

        def gu_pieces(i):
            for q in range(4):
                pieces.append((i, "g", q))
                pieces.append((i, "u", q))

        def d_pieces(i):
            for r in range(4):
                pieces.append((i, "d", r))

        gu_pieces(0)
        for i in range(len(items)):
            if i + 1 < len(items):
                gu_pieces(i + 1)
            d_pieces(i)
        state = {"issued": 0, "used": 0}
        slot_of = {}
        PRE = NSLOT - 2

        def issue_upto(n):
            while state["issued"] < min(n, len(pieces)):
                i = state["issued"]
                it, kind, q = pieces[i]
                e = items[it][1]
                s_ = i % NSLOT
                slot_of[pieces[i]] = s_
                if kind == "g":
                    src = k.wgu_d[l, e].rearrange("(kc p) f -> p kc f", p=128)[:, :, q * PW:(q + 1) * PW]
                elif kind == "u":
                    src = k.wgu_d[l, e].rearrange("(kc p) f -> p kc f", p=128)[:, :, 1024 + q * PW:1024 + (q + 1) * PW]
                else:
                    src = k.wdown_d[l, e].rearrange("(kc p) f -> p kc f", p=128)[:, :, q * PW:(q + 1) * PW]
                dma(P, "pool", ring[s_][:], src, [], [ringres[s_]])
                state["issued"] += 1

        def take(key):
            issue_upto(state["used"] + PRE)
            state["used"] += 1
            return slot_of[key]

        nt = [0]
        pend_bc = []

        def GU(i):
            gi, e = items[i]
            grp = groups[gi]
            gt0 = grp[0][0]
            hb = i % 2
            for li, (t0, tn, ci) in enumerate(grp):
                ps, pr = getps(k)
                mm(P, ps[:, :tn], k.identf[0:NE, e:e + 1].broadcast_to([NE, 128]), GT[:, t0:t0 + tn], True, True, [k.cres, gtres[ci]], [pr])
                act(P, gb[hb][:, t0 - gt0:t0 - gt0 + tn], ps[:, :tn], AF.Copy, [pr], [gbres[hb][li]], scale=1.0 / ALPHA)
            for q in range(4):
                sg_ = take((i, "g", q))
                su_ = take((i, "u", q))
                for j in range(2):
                    fc = 2 * q + j
                    vb = V_BGU + (l * NE + e) * 16
                    bgc = k.vecs[:, vb + fc:vb + fc + 1]
                    buc = k.vecs[:, vb + 8 + fc:vb + 8 + fc + 1]
                    for li, (t0, tn, ci) in enumerate(grp):
                        lo = t0 - gt0
                        psg, prg = getps(k)
                        for kc in range(KC):
                            mm(P, psg[:, :tn], ring[sg_][:, kc, j * 128:(j + 1) * 128], k.xT[:, kc, t0:t0 + tn], kc == 0, kc == KC - 1,
                               [ringres[sg_], k.xres[kc][ci]], [prg])
                        psu, pru = getps(k)
                        for kc in range(KC):
                            mm(P, psu[:, :tn], ring[su_][:, kc, j * 128:(j + 1) * 128], k.xT[:, kc, t0:t0 + tn], kc == 0, kc == KC - 1,
                               [ringres[su_], k.xres[kc][ci]], [pru])
                        s_ = nt[0] % NT
                        nt[0] += 1
                        R = [tres[s_]]
                        ts(P, "dve", gc[s_][:, :tn], psg[:, :tn], bgc, LIMIT, ALU.add, ALU.min, [prg, k.cres], R)
                        act(P, gc[s_][:, :tn], gc[s_][:, :tn], AF.Silu, R, R, scale=ALPHA)
                        act(P, u0[s_][:, :tn], psu[:, :tn], AF.Identity, [pru, k.cres], R, bias=buc)

                        def stage_bc(s_=s_, tn=tn, lo=lo, hb=hb, li=li, fc=fc, R=R):
                            ts(P, "pool", u0[s_][:, :tn], u0[s_][:, :tn], LIMIT, -LIMIT, ALU.min, ALU.max, R, R)
                            tt(P, "dve", gc[s_][:, :tn], gc[s_][:, :tn], gb[hb][:, lo:lo + tn], ALU.mult, R + [gbres[hb][li]], R)
                            stt(P, hm[hb][:, fc, lo:lo + tn], u0[s_][:, :tn], 1.0, gc[s_][:, :tn], ALU.add, ALU.mult, R, [hmres[hb][fc][li]])

                        if len(pend_bc) >= SKEW:
                            pend_bc.pop(0)()
                        pend_bc.append(stage_bc)

        def DOWN(i):
            gi, e = items[i]
            grp = groups[gi]
            gt0 = grp[0][0]
            hb = i % 2
            for r in range(4):
                sd_ = take((i, "d", r))
                for j in range(2):
                    dc = 2 * r + j
                    for li, (t0, tn, ci) in enumerate(grp):
                        lo = t0 - gt0
                        w = 1 if ci == 0 else 0
                        ps, pr = getps(k)
                        for fc in range(KC):
                            mm(P, ps[:, :tn], ring[sd_][:, fc, j * 128:(j + 1) * 128], hm[hb][:, fc, lo:lo + tn], fc == 0, fc == KC - 1,
                               [ringres[sd_], hmres[hb][fc][li]], [pr])
                        stt(P, k.hT[:, dc, t0:t0 + tn], ps[:, :tn], k.mod[l][:, w, 40 + dc:41 + dc], k.hT[:, dc, t0:t0 + tn],
                            ALU.mult, ALU.add, [pr, k.modres, k.hres[dc][ci]], [k.hres[dc][ci]])

        GU(0)
        for i in range(len(items)):
            if i + 1 < len(items):
                GU(i + 1)
            else:
                while pend_bc:
                    pend_bc.pop(0)()
            DOWN(i)
        P.barrier()


def wload(k, dst, Wd, c0, n, res):
    src = Wd.rearrange("(kc p) f -> p kc f", p=128)[:, :, c0:c0 + n]
    return dma(k.P, "pool", dst, src, [], [res])


def fm_proj(k, ps_ap, pr, wt, wres, c0, n, ci, t0, tn):
    for kc in range(KC):
        mm(k.P, ps_ap, wt[:, kc, c0:c0 + n], k.xT[:, kc, t0:t0 + tn], kc == 0, kc == KC - 1, [wres, k.xres[kc][ci]], [pr])


def rstd_from_ps(k, dst, ps_ap, pr, dres, n_inv):
    P = k.P
    npart = dst.shape[0]
    act(P, dst, ps_ap, AF.Ln, [pr], [dres], bias=k.epsc[0:npart, 0:1], scale=n_inv)
    act(P, dst, dst, AF.Exp, [dres], [dres], scale=-0.5)


def qk_prep(k, name, Wd, Wswd, c0, nf, gcol, gscol, ones_ap, hd, rope_i, dst, dres_fn, with_ctx, post_scale=1.0, norm=True, dbuf=True):
    P, nc = k.P, k.nc
    with contextlib.ExitStack() as st:
        wt = st.enter_context(_sb(nc, name + "w", [128, KC, nf], BF16))
        wts = st.enter_context(_sb(nc, name + "ws", [128, KC, nf], BF16))
        wres = Res()
        wload(k, wt[:], Wd, c0, nf, wres)
        wload(k, wts[:], Wswd, c0, nf, wres)
        tl = [[st.enter_context(_sb(nc, name + "t%d%d" % (i, j), [nf, 512], F32)) for j in range(4)] for i in range(2 if dbuf else 1)] * (1 if dbuf else 2)
        tr_ = [Res(), Res()] if dbuf else [Res()] * 2
        rpc = [st.enter_context(_sb(nc, name + "rc%d" % i, [nf, 512], F32)) for i in range(2)]
        rps = [st.enter_context(_sb(nc, name + "rs%d" % i, [nf, 512], F32)) for i in range(2)]
        rpr = [[Res(), Res()] for _ in range(2)]
        for n_, (ci, (t0, tn)) in enumerate([(ci, c) for ci, c in enumerate(CH) if (with_ctx or ci > 0)]):
            b = n_ % 2
            sq, rs, x1, x2 = tl[b]
            R = [tr_[b]]
            lat = ci > 0
            ps1, pr1 = getps(k)
            fm_proj(k, ps1[0:nf, :tn], pr1, wt, wres, 0, nf, ci, t0, tn)
            if norm:
                act(P, sq[:, :tn], ps1[0:nf, :tn], AF.Square, [pr1], R)
                pss, prs = getps(k)
                mm(P, pss[0:nf, :tn], ones_ap, sq[:, :tn], True, True, R + [k.cres], [prs])
                rstd_from_ps(k, rs[:, :tn], pss[0:nf, :tn], prs, tr_[b], 1.0 / hd)
                stt(P, x1[:, :tn], ps1[0:nf, :tn], gcol, rs[:, :tn], ALU.mult, ALU.mult, [pr1, k.cres] + R, R)
            else:
                act(P, x1[:, :tn], ps1[0:nf, :tn], AF.Copy, [pr1], R, scale=post_scale)
            dcol = t0 if with_ctx else t0 - NCTX
            if lat:
                ps2, pr2 = getps(k)
                fm_proj(k, ps2[0:nf, :tn], pr2, wts, wres, 0, nf, ci, t0, tn)
                if norm:
                    stt(P, x2[:, :tn], ps2[0:nf, :tn], gscol, rs[:, :tn], ALU.mult, ALU.mult, [pr2, k.cres] + R, R)
                else:
                    act(P, x2[:, :tn], ps2[0:nf, :tn], AF.Copy, [pr2], R, scale=post_scale)
                pb = n_ % 2
                dma(P, "sp", rpc[pb][:, :tn], k.rope_d[rope_i, 0:nf, t0 - NCTX:t0 - NCTX + tn], [], [rpr[pb][0]])
                dma(P, "sp", rps[pb][:, :tn], k.rope_d[rope_i + 1, 0:nf, t0 - NCTX:t0 - NCTX + tn], [], [rpr[pb][1]])
                tt(P, "dve", x1[:, :tn], x1[:, :tn], rpc[pb][:, :tn], ALU.mult, R + [rpr[pb][0]], R)
                tt(P, "dve", x2[:, :tn], x2[:, :tn], rps[pb][:, :tn], ALU.mult, R + [rpr[pb][1]], R)
                tt(P, "dve", dst[:, dcol:dcol + tn], x1[:, :tn], x2[:, :tn], ALU.add, R, [dres_fn(ci)])
            else:
                cp(P, "dve", dst[:, dcol:dcol + tn], x1[:, :tn], R, [dres_fn(ci)])
        P.barrier()


def v_prep(k, name, Wd, c0, nv, dst, dres, rows=128):
    P, nc = k.P, k.nc
    with contextlib.ExitStack() as st:
        wt = st.enter_context(_sb(nc, name + "w", [128, KC, nv], BF16))
        wres = Res()
        wload(k, wt[:], Wd, c0, nv, wres)
        for ti in range(T // rows):
            ci, _ = chunk_of(ti * rows)
            ps, pr = getps(k)
            for kc in range(KC):
                mm(P, ps[0:rows, 0:nv], k.xT[:, kc, ti * rows:(ti + 1) * rows], wt[:, kc, :], kc == 0, kc == KC - 1,
                   [wres, k.xres[kc][ci]], [pr])
            if ti % 2 == 0:
                cp(P, "dve", dst[0:rows, ti, :], ps[0:rows, 0:nv], [pr], [dres])
            else:
                act(P, dst[0:rows, ti, :], ps[0:rows, 0:nv], AF.Copy, [pr], [dres])
        P.barrier()


def attn_core(k, KT_fn, QT_fn, V_fn, kdim_reads, scale, onesb, pt, ptres, qi, out_cb):
    P = k.P
    a = k.acc_next
    k.acc_next = (a + 1) % 2
    psO, prO = k.ps[4 + 2 * a], k.psr[4 + 2 * a]
    psL, prL = k.ps[5 + 2 * a], k.psr[5 + 2 * a]
    NTL = T // 128
    pend = []

    def issue_s(jt):
        psS, prS = getps(k)
        mm(P, psS[:, :], KT_fn(jt), QT_fn(qi), True, True, kdim_reads, [prS])
        s = k.pt_next
        k.pt_next = (s + 1) % len(pt)
        act(P, pt[s][:, :], psS[:, :], AF.Exp, [prS], [ptres[s]], scale=scale)
        pend.append((jt, s))

    def issue_o():
        jt, s = pend.pop(0)
        mm(P, psO[:, :], V_fn(jt), pt[s][:, :], jt == 0, jt == NTL - 1, [ptres[s]] + kdim_reads, [prO])
        mm(P, psL[:, :], onesb[:], pt[s][:, :], jt == 0, jt == NTL - 1, [ptres[s], k.cres], [prL])

    LOOK = 2
    for jt in range(NTL):
        issue_s(jt)
        if len(pend) > LOOK:
            issue_o()
    while pend:
        issue_o()
    out_cb(qi, psO, prO, psL, prL)


def out_proj_head(k, l, Wo_d, r0, nrows, y_ap_fn, yres_fn, chunk_list, name):
    P, nc = k.P, k.nc
    with contextlib.ExitStack() as st:
        wo = st.enter_context(_sb(nc, name + "wo", [nrows, D], BF16))
        wres = Res()
        dma(P, "pool", wo[:], Wo_d[r0:r0 + nrows, :], [], [wres])
        for dc in range(KC):
            for (ci, t0, tn, yc0) in chunk_list:
                w = 1 if ci == 0 else 0
                ps, pr = getps(k)
                mm(P, ps[:, :tn], wo[:, dc * 128:(dc + 1) * 128], y_ap_fn(yc0, tn), True, True, [wres, yres_fn(ci)], [pr])
                stt(P, k.hT[:, dc, t0:t0 + tn], ps[:, :tn], k.mod[l][:, w, 16 + dc:17 + dc], k.hT[:, dc, t0:t0 + tn], ALU.mult, ALU.add,
                    [pr, k.modres, k.hres[dc][ci]], [k.hres[dc][ci]])
        P.barrier()


def attention_mixer(k, l):
    P, nc = k.P, k.nc
    lam_init = 0.8 - 0.6 * math.exp(-0.3 * l)
    W, Wsw, Wo = k.attw_d, k.attwsw_d, k.attwo_d
    VA = V_ATT
    col = lambda i: k.vecs[:, VA + i:VA + i + 1]
    k.ps_n = 4
    k.ps_next = 0
    k.acc_next = 0
    k.pt_next = 0
    with contextlib.ExitStack() as st:
        onesb = st.enter_context(_sb(nc, "onesb", [128, 128], BF16))
        cp(P, "dve", onesb[:], k.onesf, [k.cres], [k.cres])
        lp = st.enter_context(_sb(nc, "lp", [128, 4, 64], F32))
        lpp = st.enter_context(_sb(nc, "lpp", [128, 2, 64], F32))
        lsm = st.enter_context(_sb(nc, "lsm", [128, 4], F32))
        dgc = st.enter_context(_sb(nc, "dgc", [128, 1], F32))
        lres = Res()
        dma(P, "sp", lp[:], k.dlam_d[:, :, :], [], [lres])
        tt(P, "dve", lpp[:], lp[:, 0::2, :], lp[:, 1::2, :], ALU.mult, [lres], [lres])
        P.dve(lambda e: e.reduce_sum(out=lsm[:, 0:2], in_=lpp[:], axis=AX.X), [lres], [lres])
        act(P, lsm[:, 0:2], lsm[:, 0:2], AF.Exp, [lres], [lres])
        tt(P, "dve", lsm[:, 2:3], lsm[:, 1:2], lsm[:, 0:1], ALU.subtract, [lres], [lres])
        ts(P, "dve", lsm[:, 3:4], lsm[:, 2:3], -lam_init, None, ALU.add, None, [lres], [lres])
        nlam = lsm[:, 3:4]
        ts(P, "dve", dgc[:], col(8), 1.0 - lam_init, None, ALU.mult, None, [k.cres], [lres])

        YT = st.enter_context(_sb(nc, "YT", [128, KC, NLAT], BF16))
        yres = [[Res() for _ in range(4)] for _ in range(KC)]
        pt = [st.enter_context(_sb(nc, "pt%d" % i, [128, 512], BF16)) for i in range(3)]
        ptres = [Res() for _ in range(3)]
        rl = [st.enter_context(_sb(nc, "rl%d" % i, [128, 512], F32)) for i in range(2)]
        rlres = [Res() for _ in range(2)]
        rln = [0]

        for kv in range(2):
            with contextlib.ExitStack() as s2:
                KT = s2.enter_context(_sb(nc, "gKT%d" % kv, [128, T], BF16))
                ktres = [Res() for _ in CH]
                Vt = s2.enter_context(_sb(nc, "gV%d" % kv, [128, T // 128, 128], BF16))
                vres = Res()
                QT = [s2.enter_context(_sb(nc, "gQT%d_%d" % (kv, i), [128, NLAT], BF16)) for i in range(2)]
                qres = [[Res() for _ in CH] for _ in range(2)]
                k.ps_n = 8
                qk_prep(k, "gk%d" % kv, W, Wsw, 512 + kv * 128, 128, col(2), col(3), k.onesf, 128, 0, KT, (lambda ci: ktres[ci]), True)
                v_prep(k, "gv%d" % kv, W, 768 + kv * 128, 128, Vt, vres)
                for g in range(2):
                    h = kv * 2 + g
                    qk_prep(k, "gq%d" % h, W, Wsw, h * 128, 128, col(0), col(1), k.onesf, 128, 0, QT[g], (lambda ci, g=g: qres[g][ci]), False)
                if kv == 0 and "KTd" in k.dbg:
                    k.dbg_ops.append(dma(P, "pool", k.dbg["KTd"], KT[:], ktres, []))
                    k.dbg_ops.append(dma(P, "pool", k.dbg["QTd"], QT[0][:], qres[0], []))
                    k.dbg_ops.append(dma(P, "pool", k.dbg["VTd"].rearrange("p (a b) -> p a b", b=128), Vt[:], [vres], []))
                    k.dbg_ops.append(dma(P, "pool", k.dbg["XTd"], k.xT[:, 0, :], [k.xres[0][ci] for ci in range(5)], []))
                k.ps_n = 4
                for g in range(2):
                    h = kv * 2 + g
                    for qi in range(4):
                        def cb(qi, psO, prO, psL, prL, h=h):
                            s = rln[0] % 2
                            rln[0] += 1
                            P.dve(lambda e, o=rl[s][:, :], i=psL[:, :]: e.reciprocal(out=o, in_=i), [prL], [rlres[s]])
                            tt(P, "dve", YT[:, h, qi * 512:(qi + 1) * 512], psO[:, :], rl[s][:, :], ALU.mult, [prO, rlres[s]], [yres[h][qi]])
                        attn_core(k, (lambda jt: KT[:, jt * 128:(jt + 1) * 128]), (lambda qi, g=g: QT[g][:, qi * 512:(qi + 1) * 512]),
                                  (lambda jt: Vt[:, jt, :]), ktres + qres[g] + [vres], 128 ** -0.5, onesb, pt, ptres, qi, cb)
                P.barrier()

        with contextlib.ExitStack() as s3:
            o0 = s3.enter_context(_sb(nc, "do0", [128, 512], F32))
            od = s3.enter_context(_sb(nc, "dod", [128, 512], F32))
            dsq = s3.enter_context(_sb(nc, "dsq", [128, 512], F32))
            ores = Res()
            for h in range(4):
                with contextlib.ExitStack() as s2:
                    KT = s2.enter_context(_sb(nc, "dKT%d" % h, [128, T], BF16))
                    ktres = [Res() for _ in CH]
                    Vt = s2.enter_context(_sb(nc, "dV%d" % h, [128, T // 128, 128], BF16))
                    vres = Res()
                    QT = s2.enter_context(_sb(nc, "dQT%d" % h, [128, NLAT], BF16))
                    qres = [Res() for _ in CH]
                    k.ps_n = 8
                    qk_prep(k, "dk%d" % h, W, Wsw, 1536 + h * 128, 128, col(6), col(7), k.ones2, 64, 2, KT, (lambda ci: ktres[ci]), True)
                    v_prep(k, "dv%d" % h, W, 2048 + h * 128, 128, Vt, vres)
                    qk_prep(k, "dq%d" % h, W, Wsw, 1024 + h * 128, 128, col(4), col(5), k.ones2, 64, 2, QT, (lambda ci: qres[ci]), False)
                    k.ps_n = 4
                    for qi in range(4):
                        for half in range(2):
                            def cb(qi, psO, prO, psL, prL, h=h, half=half):
                                s = rln[0] % 2
                                rln[0] += 1
                                P.dve(lambda e, o=rl[s][:, :], i=psL[:, :]: e.reciprocal(out=o, in_=i), [prL], [rlres[s]])
                                if half == 0:
                                    tt(P, "dve", o0[:, :], psO[:, :], rl[s][:, :], ALU.mult, [prO, rlres[s]], [ores])
                                    return
                                tt(P, "dve", od[:, :], psO[:, :], rl[s][:, :], ALU.mult, [prO, rlres[s], ores], [ores])
                                stt(P, od[:, :], od[:, :], nlam, o0[:, :], ALU.mult, ALU.add, [ores, lres], [ores])
                                act(P, dsq[:, :], od[:, :], AF.Square, [ores], [ores])
                                pss, prs = getps(k)
                                mm(P, pss[:, :], k.onesf, dsq[:, :], True, True, [ores, k.cres], [prs])
                                rstd_from_ps(k, dsq[:, :], pss[:, :], prs, ores, 1.0 / 128)
                                stt(P, YT[:, 4 + h, qi * 512:(qi + 1) * 512], od[:, :], dgc[:, 0:1], dsq[:, :], ALU.mult, ALU.mult,
                                    [ores, lres], [yres[4 + h][qi]])
                            p0 = half * 64
                            attn_core(k, (lambda jt, p0=p0: KT[p0:p0 + 64, jt * 128:(jt + 1) * 128]),
                                      (lambda qi, p0=p0: QT[p0:p0 + 64, qi * 512:(qi + 1) * 512]),
                                      (lambda jt: Vt[:, jt, :]), ktres + qres + [vres], 64 ** -0.5, onesb, pt, ptres, qi, cb)
                    P.barrier()

        if "YTd" in k.dbg:
            k.dbg_ops.append(dma(P, "pool", k.dbg["YTd"].rearrange("p (a b) -> p a b", a=KC), YT[:], [r for rr_ in yres for r in rr_], []))
        k.ps_n = 8
        chunk_list = [(ci, CH[ci][0], CH[ci][1], CH[ci][0] - NCTX) for ci in range(1, 5)]
        with contextlib.ExitStack() as so:
            wo = [so.enter_context(_sb(nc, "awo%d" % i, [128, KC, 512], BF16)) for i in range(2)]
            wores = [Res(), Res()]
            for half in range(2):
                dma(P, "pool", wo[half][:], Wo.rearrange("(hc p) d -> p hc d", p=128)[:, :, half * 512:(half + 1) * 512], [], [wores[half]])
            for dc in range(KC):
                half, dcl = dc // 4, dc % 4
                for (ci, t0, tn, yc0) in chunk_list:
                    ps, pr = getps(k)
                    for hc in range(KC):
                        mm(P, ps[:, :tn], wo[half][:, hc, dcl * 128:(dcl + 1) * 128], YT[:, hc, yc0:yc0 + tn], hc == 0, hc == KC - 1,
                           [wores[half], yres[hc][ci - 1]], [pr])
                    stt(P, k.hT[:, dc, t0:t0 + tn], ps[:, :tn], k.mod[l][:, 0, 16 + dc:17 + dc], k.hT[:, dc, t0:t0 + tn], ALU.mult, ALU.add,
                        [pr, k.modres, k.hres[dc][ci]], [k.hres[dc][ci]])
            P.barrier()
    k.ps_n = 8


class Chain:
    pass


def scan_chains(k, chains, dk):
    P = k.P
    NCH = T // 64
    pend = {}
    for step in range(NCH + 1):
        cur = {}
        if step < NCH:
            for ci_, ch in enumerate(chains):
                n = ch.order[step]
                t0 = n * 64
                psa, pra = getps(k)
                mm(P, psa[0:64, 0:64], ch.kin[0:dk, t0:t0 + 64], ch.qin[0:dk, t0:t0 + 64], True, True, [ch.kres, ch.qres], [pra])
                psk, prk = getps(k)
                mm(P, psk[0:dk, 0:128], ch.kst[0:64, n, 0:dk], ch.v[0:64, n, :], True, True, [ch.kstres, ch.vres], [prk])
                cur[ci_] = (n, t0, psa, pra, psk, prk)
        outs = {}
        for ci_, ch in enumerate(chains):
            if ci_ in pend:
                m, n, t0 = pend[ci_]
                pso, pro = getps(k)
                mm(P, pso[:, 0:64], ch.v[0:64, n, :], ch.att[m % 2][:, :], True, False, [ch.vres, ch.atres[m % 2]], [pro])
                mm(P, pso[:, 0:64], ch.Sb[(m - 1) % 2][0:dk, :], ch.qin[0:dk, t0:t0 + 64], False, True, [ch.sbres[(m - 1) % 2], ch.qres], [pro])
                outs[ci_] = (n, t0, pso, pro)
        for ci_, ch in enumerate(chains):
            if ci_ in cur:
                n, t0, psa, pra, psk, prk = cur[ci_]
                b = step % 2
                tt(P, "dve", ch.att[b][:, :], psa[0:64, 0:64], ch.mask, ALU.mult, [pra, k.cres], [ch.atres[b]])
            if ci_ in outs:
                n_, t0_, pso, pro = outs[ci_]
                tt(P, "dve", ch.OT[:, t0_:t0_ + 64], pso[:, 0:64], ch.OT[:, t0_:t0_ + 64], ALU.add, [pro, ch.otres[n_]], [ch.otres[n_]])
            if ci_ in cur:
                n, t0, psa, pra, psk, prk = cur[ci_]
                stt(P, ch.S32[0:dk, :], ch.S32[0:dk, :], ch.dec_fn(n), psk[0:dk, 0:128], ALU.mult, ALU.add,
                    [prk, ch.s32res, ch.decres], [ch.s32res])
                act(P, ch.Sb[step % 2][0:dk, :], ch.S32[0:dk, :], AF.Copy, [ch.s32res], [ch.sbres[step % 2]])
        pend = {ci_: (step, cur[ci_][0], cur[ci_][1]) for ci_ in cur}


def make_chain(k, st, name, d, dk, qin, kin, kst, v, OT, otres, qres, kres, kstres, vres, dec_fn, decres, msk):
    nc, P = k.nc, k.P
    ch = Chain()
    ch.order = list(range(T // 64)) if d == 0 else [3, 2, 1, 0] + list(range(T // 64 - 1, 3, -1))
    ch.qin, ch.kin, ch.kst, ch.v, ch.OT, ch.otres = qin, kin, kst, v, OT, otres
    ch.qres, ch.kres, ch.kstres, ch.vres, ch.dec_fn, ch.decres = qres, kres, kstres, vres, dec_fn, decres
    ch.mask = msk[:, d, :]
    ch.att = [st.enter_context(_sb(nc, name + "att%d" % i, [64, 64], BF16)) for i in range(2)]
    ch.atres = [Res() for _ in range(2)]
    ch.S32 = st.enter_context(_sb(nc, name + "S32", [128, 128], F32))
    ch.Sb = [st.enter_context(_sb(nc, name + "Sb%d" % i, [128, 128], BF16)) for i in range(2)]
    ch.s32res, ch.sbres = Res(), [Res(), Res()]
    P.dve(lambda e: e.memset(ch.S32[:], 0.0), [], [ch.s32res])
    P.dve(lambda e: e.memset(ch.Sb[0][:], 0.0), [], [ch.sbres[0]])
    P.dve(lambda e: e.memset(ch.Sb[1][:], 0.0), [], [ch.sbres[1]])
    return ch


def head_finish(k, l, st, name, OT, otres, gT, gres, gain_col, centered, Wo, row0):
    P, nc = k.P, k.nc
    yT = st.enter_context(_sb(nc, name + "yT", [128, T], BF16))
    yres = [Res() for _ in CH]
    sq = st.enter_context(_sb(nc, name + "fsq", [128, 512], F32))
    xc = st.enter_context(_sb(nc, name + "fxc", [128, 512], F32))
    sq2 = st.enter_context(_sb(nc, name + "fsq2", [128, 512], F32))
    xc2 = st.enter_context(_sb(nc, name + "fxc2", [128, 512], F32))
    bufs = [(sq, xc, Res()), (sq2, xc2, Res())]
    keep = {}

    def stage1(ci):
        t0, tn = CH[ci]
        sq_, xc_, R = bufs[ci % 2]
        ors = [otres[n] for n in range(t0 // 64, (t0 + tn) // 64)]
        src = OT[:, t0:t0 + tn]
        if centered:
            psm, prm = getps(k)
            mm(P, psm[:, :tn], k.onesf, src, True, True, ors + [k.cres], [prm])
            stt(P, xc_[:, :tn], psm[:, :tn], -1.0 / 128, src, ALU.mult, ALU.add, [prm] + ors, [R])
            src = xc_[:, :tn]
            ors = [R]
        act(P, sq_[:, :tn], src, AF.Square, ors, [R])
        pss, prs = getps(k)
        mm(P, pss[:, :tn], k.onesf, sq_[:, :tn], True, True, [R, k.cres], [prs])
        rstd_from_ps(k, sq_[:, :tn], pss[:, :tn], prs, R, 1.0 / 128)
        keep[ci] = (src, ors)

    def stage2(ci):
        t0, tn = CH[ci]
        sq_, xc_, R = bufs[ci % 2]
        src, ors = keep[ci]
        stt(P, xc_[:, :tn], src, gain_col, sq_[:, :tn], ALU.mult, ALU.mult, ors + [R, k.cres], [R])
        tt(P, "dve", yT[:, t0:t0 + tn], xc_[:, :tn], gT[:, t0:t0 + tn], ALU.mult, [R, gres], [yres[ci]])

    stage1(0)
    for ci in range(len(CH)):
        if ci + 1 < len(CH):
            stage1(ci + 1)
        stage2(ci)
    chunk_list = [(ci, CH[ci][0], CH[ci][1], CH[ci][0]) for ci in range(len(CH))]
    out_proj_head(k, l, Wo, row0, 128, (lambda yc0, tn: yT[:, yc0:yc0 + tn]), (lambda ci: yres[ci]), chunk_list, name)


def recurrent_mixer(k, l):
    P, nc = k.P, k.nc
    W, Wsw, Wo = k.recw_d, k.recwsw_d, k.recwo_d
    NCH = T // 64
    with contextlib.ExitStack() as st0:
        msk = st0.enter_context(_sb(nc, "rmsk", [64, 2, 64], F32))
        rmask = st0.enter_context(_sb(nc, "rrmask", [128, 512], F32))
        rtab = st0.enter_context(_sb(nc, "rtab_sb", [128, 24, 64], F32))
        rdec = st0.enter_context(_sb(nc, "rdec_sb", [128, 4], F32))
        lbe = st0.enter_context(_sb(nc, "lbe", [128, 8, 3], F32))
        lbs = st0.enter_context(_sb(nc, "lbs", [128, 8], F32))
        lbv = st0.enter_context(_sb(nc, "lbv", [128, 8], F32))
        omlv = st0.enter_context(_sb(nc, "omlv", [128, 8], F32))
        rc = Res()
        dma(P, "sp", msk[:], k.masks_d[:, :, :], [], [rc])
        dma(P, "sp", rtab[:], k.rtab_d[:, :, :], [], [rc])
        dma(P, "sp", rdec[:], k.rdec_d[:, :], [], [rc])
        P.dve(lambda e: e.memset(rmask[:], 1.0), [], [rc])
        P.dve(lambda e: e.memset(rmask[:, :].rearrange("p (n c) -> p n c", c=64)[:, :, 0:1], 0.0), [rc], [rc])
        act(P, lbe[:], k.vecs[:, V_REC:V_REC + 24].rearrange("p (a s) -> p a s", s=3), AF.Exp, [k.cres], [rc])
        P.dve(lambda e: e.reduce_sum(out=lbs[:], in_=lbe[:], axis=AX.X), [rc], [rc])
        P.dve(lambda e: e.reciprocal(out=lbs[:], in_=lbs[:]), [rc], [rc])
        P.dve(lambda e: e.reduce_sum(out=lbv[:], in_=lbe[:, :, 0:l + 1], axis=AX.X), [rc], [rc])
        tt(P, "dve", lbv[:], lbv[:], lbs[:], ALU.mult, [rc], [rc])
        ts(P, "dve", omlv[:], lbv[:], -1.0, 1.0, ALU.mult, ALU.add, [rc], [rc])

        for hh in range(4):
            with contextlib.ExitStack() as st:
                nm = "hg%d" % hh
                qin = [st.enter_context(_sb(nc, nm + "qin%d" % d, [128, T], BF16)) for d in range(2)]
                kin = [st.enter_context(_sb(nc, nm + "kin%d" % d, [128, T], BF16)) for d in range(2)]
                kst = [st.enter_context(_sb(nc, nm + "kst%d" % d, [64, NCH, 128], BF16)) for d in range(2)]
                dec = [st.enter_context(_sb(nc, nm + "dec%d" % d, [128, NCH], F32)) for d in range(2)]
                v = st.enter_context(_sb(nc, nm + "v", [64, NCH, 128], BF16))
                gT = st.enter_context(_sb(nc, nm + "gT", [128, T], BF16))
                qres, kres, kstres, decres = [Res(), Res()], [Res(), Res()], [Res(), Res()], [Res(), Res()]
                vres, gres = Res(), Res()
                v_prep(k, nm + "v", W, 1536 + hh * 128, 128, v, vres, rows=64)
                with contextlib.ExitStack() as s2:
                    wt = s2.enter_context(_sb(nc, nm + "w", [128, KC, 4, 128], BF16))
                    wres = Res()
                    for i, c0 in enumerate((hh * 128, 512 + hh * 128, 1024 + hh * 128, 2048 + hh * 128)):
                        wload(k, wt[:, :, i, :], W, c0, 128, wres)
                    tmp = [s2.enter_context(_sb(nc, nm + "tmp%d" % i, [128, 512], F32)) for i in range(8)]
                    qf, fg, lf, kk, bs, b2, ee, kT = tmp
                    R = Res()
                    for ci, (t0, tn) in enumerate(CH):
                        n0, nn = t0 // 64, tn // 64
                        psq, prq = getps(k)
                        fm_proj(k, psq[:, :tn], prq, wt[:, :, 0, :], wres, 0, 128, ci, t0, tn)
                        act(P, qf[:, :tn], psq[:, :tn], AF.Silu, [prq], [R])
                        psg, prg = getps(k)
                        fm_proj(k, psg[:, :tn], prg, wt[:, :, 3, :], wres, 0, 128, ci, t0, tn)
                        act(P, gT[:, t0:t0 + tn], psg[:, :tn], AF.Silu, [prg], [gres])
                        for d in range(2):
                            j = d * 4 + hh
                            psf, prf = getps(k)
                            fm_proj(k, psf[:, :tn], prf, wt[:, :, 1 + d, :], wres, 0, 128, ci, t0, tn)
                            act(P, fg[:, :tn], psf[:, :tn], AF.Sigmoid, [prf], [R])
                            ts(P, "dve", fg[:, :tn], fg[:, :tn], omlv[:, j:j + 1], lbv[:, j:j + 1], ALU.mult, ALU.add, [R, rc], [R])
                            act(P, lf[:, :tn], fg[:, :tn], AF.Ln, [R], [R])
                            ts(P, "dve", kk[:, :tn], fg[:, :tn], -1.0, 1.0, ALU.mult, ALU.add, [R], [R])
                            P.dve(lambda e, o=bs[:, :tn], m=rmask[:, :tn], x=lf[:, :tn]: e.tensor_tensor_scan(
                                out=o, data0=m, data1=x, initial=0.0, op0=ALU.mult, op1=ALU.add), [R, rc], [R])
                            bs3 = bs[:, :tn].rearrange("p (n c) -> p n c", c=64)
                            totb = bs3[:, :, 63:64].broadcast_to([128, nn, 64])
                            act(P, dec[d][:, n0:n0 + nn], bs3[:, :, 63], AF.Exp, [R], [decres[d]])
                            v3 = lambda t: t[:, :tn].rearrange("p (n c) -> p n c", c=64)
                            tt(P, "dve", v3(b2), totb, bs3, ALU.subtract, [R], [R])
                            if d == 0:
                                bcur = bs
                                act(P, ee[:, :tn], b2[:, :tn], AF.Exp, [R], [R])
                            else:
                                tt(P, "dve", ee[:, :tn], bs[:, :tn], lf[:, :tn], ALU.subtract, [R], [R])
                                act(P, ee[:, :tn], ee[:, :tn], AF.Exp, [R], [R])
                                tt(P, "dve", b2[:, :tn], b2[:, :tn], lf[:, :tn], ALU.add, [R], [R])
                                bcur = b2
                            tt(P, "dve", kT[:, :tn], kk[:, :tn], ee[:, :tn], ALU.mult, [R], [R])
                            for g4 in range(0, nn, 4):
                                pst, prt = getps(k)
                                m4 = min(4, nn - g4)
                                for jj in range(m4):
                                    tr(P, pst[0:64, jj * 128:(jj + 1) * 128], kT[:, (g4 + jj) * 64:(g4 + jj + 1) * 64], k.identf, [R, k.cres], [prt])
                                cp(P, "dve", kst[d][0:64, n0 + g4:n0 + g4 + m4, :], pst[0:64, 0:m4 * 128].rearrange("p (a f) -> p a f", f=128),
                                   [prt], [kstres[d]])
                            act(P, ee[:, :tn], bcur[:, :tn], AF.Exp, [R], [R])
                            stt(P, qin[d][:, t0:t0 + tn], qf[:, :tn], 128 ** -0.5, ee[:, :tn], ALU.mult, ALU.mult, [R], [qres[d]])
                            act(P, ee[:, :tn], bcur[:, :tn], AF.Exp, [R], [R], scale=-1.0)
                            tt(P, "dve", kin[d][:, t0:t0 + tn], kk[:, :tn], ee[:, :tn], ALU.mult, [R], [kres[d]])
                    P.barrier()
                OT = st.enter_context(_sb(nc, nm + "OT", [128, T], F32))
                otres = [Res() for _ in range(NCH)]
                P.pool(lambda e, o=OT: e.memset(o[:], 0.0), [], otres)
                chains = [make_chain(k, st, nm + "c%d" % d, d, 128, qin[d], kin[d], kst[d], v, OT, otres, qres[d], kres[d], kstres[d], vres,
                                     (lambda n, d=d: dec[d][:, n:n + 1]), decres[d], msk) for d in range(2)]
                scan_chains(k, chains, 128)
                head_finish(k, l, st, nm, OT, otres, gT, gres, k.vecs[:, V_REC + 24:V_REC + 25], False, Wo, hh * 128)
                P.barrier()

        for r in range(4):
            with contextlib.ExitStack() as st:
                nm = "rt%d" % r
                qin = [st.enter_context(_sb(nc, nm + "qin%d" % d, [64, T], BF16)) for d in range(2)]
                kin = [st.enter_context(_sb(nc, nm + "kin%d" % d, [64, T], BF16)) for d in range(2)]
                kst = [st.enter_context(_sb(nc, nm + "kst%d" % d, [64, NCH, 64], BF16)) for d in range(2)]
                v = st.enter_context(_sb(nc, nm + "v", [64, NCH, 128], BF16))
                gT = st.enter_context(_sb(nc, nm + "gT", [128, T], BF16))
                qres, kres, kstres = [Res(), Res()], [Res(), Res()], [Res(), Res()]
                vres, gres = Res(), Res()
                v_prep(k, nm + "v", W, 3072 + r * 128, 128, v, vres, rows=64)
                with contextlib.ExitStack() as s2:
                    wg = s2.enter_context(_sb(nc, nm + "wg", [128, KC, 128], BF16))
                    wres = Res()
                    wload(k, wg[:], W, 3584 + r * 128, 128, wres)
                    for ci, (t0, tn) in enumerate(CH):
                        psg, prg = getps(k)
                        fm_proj(k, psg[:, :tn], prg, wg, wres, 0, 128, ci, t0, tn)
                        act(P, gT[:, t0:t0 + tn], psg[:, :tn], AF.Silu, [prg], [gres])
                    qf = s2.enter_context(_sb(nc, nm + "qf", [64, T], F32))
                    kf = s2.enter_context(_sb(nc, nm + "kf", [64, T], F32))
                    qfr = [Res() for _ in CH]
                    kfr = [Res() for _ in CH]
                    qk_prep(k, nm + "q", W, Wsw, 2560 + r * 64, 64, None, None, None, 64, 4, qf, (lambda ci: qfr[ci]), True, post_scale=1.0, norm=False, dbuf=False)
                    qk_prep(k, nm + "k", W, Wsw, 2816 + r * 64, 64, None, None, None, 64, 4, kf, (lambda ci: kfr[ci]), True, post_scale=64 ** -0.5, norm=False, dbuf=False)
                    kT = s2.enter_context(_sb(nc, nm + "kT", [64, 512], F32))
                    R = Res()
                    for d in range(2):
                        gi = r if d == 0 else 3 - r
                        tb = lambda kind, nn: rtab[0:64, (gi * 2 + d) * 3 + kind:(gi * 2 + d) * 3 + kind + 1, :].broadcast_to([64, nn, 64])
                        for ci, (t0, tn) in enumerate(CH):
                            n0, nn = t0 // 64, tn // 64
                            v3 = lambda ap: ap.rearrange("p (n c) -> p n c", c=64)
                            tt(P, "dve", v3(qin[d][:, t0:t0 + tn]), v3(qf[:, t0:t0 + tn]), tb(0, nn), ALU.mult, [qfr[ci], rc], [qres[d]])
                            tt(P, "dve", v3(kin[d][:, t0:t0 + tn]), v3(kf[:, t0:t0 + tn]), tb(1, nn), ALU.mult, [kfr[ci], rc], [kres[d]])
                            tt(P, "dve", v3(kT[:, :tn]), v3(kf[:, t0:t0 + tn]), tb(2, nn), ALU.mult, [kfr[ci], rc, R], [R])
                            pst, prt = getps(k)
                            for jj in range(nn):
                                tr(P, pst[0:64, jj * 64:(jj + 1) * 64], kT[:, jj * 64:(jj + 1) * 64], k.identf[0:64, 0:64], [R, k.cres], [prt])
                            cp(P, "dve", kst[d][0:64, n0:n0 + nn, :], pst[0:64, 0:nn * 64].rearrange("p (a f) -> p a f", f=64), [prt], [kstres[d]])
                    P.barrier()
                OT = st.enter_context(_sb(nc, nm + "OT", [128, T], F32))
                otres = [Res() for _ in range(NCH)]
                P.pool(lambda e, o=OT: e.memset(o[:], 0.0), [], otres)
                chains = []
                for d in range(2):
                    gi = r if d == 0 else 3 - r
                    chains.append(make_chain(k, st, nm + "c%d" % d, d, 64, qin[d], kin[d], kst[d], v, OT, otres, qres[d], kres[d], kstres[d], vres,
                                             (lambda n, gi=gi: rdec[0:64, gi:gi + 1]), rc, msk))
                scan_chains(k, chains, 64)
                head_finish(k, l, st, nm, OT, otres, gT, gres, k.vecs[:, V_REC + 25:V_REC + 26], True, Wo, 512 + r * 128)
                P.barrier()


def build(cfg):
    nc = bass.Bass("TRN2", target_bir_lowering=False)
    k = K()
    k.nc = nc
    k.P = P = Prog()

    def din(name, shape):
        return nc.dram_tensor(name, list(shape), F32, kind="ExternalInput").ap()

    k.x_d = din("x", [NLAT, D])
    k.ctx_d = din("ctx", [NCTX, D])
    k.cc_d = din("cc", [128, KC, 2])
    k.vecs_d = din("vecs", [128, NVEC])
    k.wada_d = din("w_ada", [DEPTH, D, 6 * D])
    k.wrouter_d = din("w_router", [DEPTH, D, NE])
    k.brouter_d = din("b_router", [DEPTH, NE])
    k.wgu_d = din("w_gu", [DEPTH, NE, D, 2 * D])
    k.wdown_d = din("w_down", [DEPTH, NE, D, D])
    k.bdown_d = din("b_down", [DEPTH, NE, D])
    k.consts_d = din("consts", [128, 3 * 128])
    k.attw_d = din("att_w", [D, 2560])
    k.attwsw_d = din("att_w_sw", [D, 2560])
    k.attwo_d = din("att_wo", [D, D])
    k.recw_d = din("rec_w", [D, 4096])
    k.recwsw_d = din("rec_w_sw", [D, 4096])
    k.recwo_d = din("rec_wo", [D, D])
    k.rope_d = din("rope", [6, 128, NLAT])
    k.dlam_d = din("dlam", [128, 4, 64])
    k.masks_d = din("masks", [64, 2, 64])
    k.rtab_d = din("rtab", [128, 24, 64])
    k.rdec_d = din("rdec", [128, 4])
    k.out_d = nc.dram_tensor("out", [NLAT, D], F32, kind="ExternalOutput").ap()
    dbg = {}
    for name, shape in cfg.get("debug", {}).items():
        dbg[name] = nc.dram_tensor(name, list(shape), F32, kind="ExternalOutput").ap()
    k.dbg = dbg
    k.dbg_ops = []

    with contextlib.ExitStack() as st:
        k.hT = st.enter_context(_sb(nc, "hT", [128, KC, T], F32))
        k.hres = [[Res() for _ in CH] for _ in range(KC)]
        k.xT = st.enter_context(_sb(nc, "xT", [128, KC, T], BF16))
        k.xres = [[Res() for _ in CH] for _ in range(KC)]
        k.vecs = st.enter_context(_sb(nc, "vecs_sb", [128, NVEC], F32))
        cst = st.enter_context(_sb(nc, "consts_sb", [128, 3 * 128], F32))
        k.identf = cst[:, 0:128]
        k.onesf = cst[:, 128:256]
        k.ones2 = cst[:, 256:384]
        k.epsc = st.enter_context(_sb(nc, "epsc", [128, 1], F32))
        k.cres = Res("consts")
        k.mod = [st.enter_context(_sb(nc, "mod%d" % l, [128, 2, 48], F32)) for l in range(DEPTH)]
        k.A1 = [st.enter_context(_sb(nc, "A1_%d" % l, [128, 2, KC], F32)) for l in range(DEPTH)]
        k.A2 = [st.enter_context(_sb(nc, "A2_%d" % l, [128, 2, KC], F32)) for l in range(DEPTH)]
        k.modres = Res("mod")
        k.ps = [st.enter_context(nc.psum_tensor("ps%d" % i, [128, 512], F32)) for i in range(8)]
        k.psr = [Res() for _ in range(8)]
        k.ps_next = 0
        k.ps_n = 8

        dma(P, "sp", k.vecs[:], k.vecs_d[:, :], [], [k.cres])
        dma(P, "sp", cst[:], k.consts_d[:, :], [], [k.cres])
        P.pool(lambda e: e.memset(k.epsc[:], EPS), [], [k.cres])

        load_tokens(k)
        modulation(k)
        final_ops = []
        for l in range(DEPTH):
            if l not in cfg.get("layer_list", range(DEPTH)):
                continue
            if "mixer" in cfg.get("stages", ()):
                norm_mod(k, l, 0)
                if l % 2 == 0:
                    recurrent_mixer(k, l)
                else:
                    attention_mixer(k, l)
            if "moe" in cfg.get("stages", ("moe",)):
                moe_layer(k, l)
            if cfg.get("layers", DEPTH) == l + 1:
                break
        for name, (kind, arg) in cfg.get("dump", {}).items():
            pass
        final_ops += store_output(k)
        final_ops += k.dbg_ops
        P.emit(nc, final_wait_ops=final_ops)
    return nc


def host_inputs(inputs, b):
    f = lambda a: np.ascontiguousarray(np.asarray(a, dtype=np.float32))
    cc = np.stack([np.asarray(inputs["c"])[b], np.asarray(inputs["c_ctx"])], axis=-1)
    cc = cc.reshape(KC, 128, 2).transpose(1, 0, 2)
    vecs = np.zeros((128, NVEC), np.float32)

    def fm(v):
        v = np.asarray(v, np.float32)
        return v.reshape(-1, 128).T

    for l in range(DEPTH):
        vecs[:, V_BADA + l * 48:V_BADA + (l + 1) * 48] = fm(inputs["b_ada"][l])
        vecs[:, V_NMIX + l * 8:V_NMIX + (l + 1) * 8] = fm(inputs["norm_mix"][l])
        vecs[:, V_NFFN + l * 8:V_NFFN + (l + 1) * 8] = fm(inputs["norm_ffn"][l])
        for e in range(NE):
            o = V_BGU + (l * NE + e) * 16
            vecs[:, o:o + 16] = fm(inputs["b_gu"][l, e])
    sw = _pairswap(128)
    g = lambda n: np.asarray(inputs[n][0], np.float32)
    qg, kg = g("att_q_gain"), g("att_k_gain")
    dq2, dk2 = np.concatenate([g("diff_q_gain")] * 2), np.concatenate([g("diff_k_gain")] * 2)
    for i, vcol in enumerate((qg, qg[sw], kg, kg[sw], dq2, dq2[sw], dk2, dk2[sw], g("diff_gain"))):
        vecs[:, V_ATT + i] = vcol
    lbl = np.asarray(inputs["rec_lb_logits"], np.float32)
    for d in range(2):
        for hh in range(4):
            for sl in range(3):
                vecs[:, V_REC + (d * 4 + hh) * 3 + sl] = lbl[sl, d, hh * 128:(hh + 1) * 128]
    vecs[:, V_REC + 24] = g("rec_hg_gain")
    vecs[:, V_REC + 25] = g("rec_ret_gain")
    consts = np.zeros((128, 3 * 128), np.float32)
    consts[:, 0:128] = np.eye(128, dtype=np.float32)
    consts[:, 128:256] = 1.0
    consts[0:64, 256:320] = 1.0
    consts[64:128, 320:384] = 1.0
    out = {
        "x": f(inputs["x"][b]), "ctx": f(inputs["ctx"][b]), "cc": f(cc), "vecs": vecs,
        "w_ada": f(inputs["w_ada"]), "w_router": f(inputs["w_router"]), "b_router": f(inputs["b_router"]),
        "w_gu": f(inputs["w_gu"]), "w_down": f(inputs["w_down"]), "b_down": f(inputs["b_down"]),
        "consts": consts,
    }
    out.update(_shared_host(inputs))
    return out


def _pairswap(n):
    p = np.arange(n)
    return p + 1 - 2 * (p % 2)


_SHARED = {}


def _shared_host(inputs):
    key = id(inputs["w_gu"])
    if _SHARED.get("key") == key:
        return _SHARED["val"]
    f = lambda a: np.ascontiguousarray(np.asarray(a, dtype=np.float32))
    aw = np.asarray(inputs["att_w_in"][0], np.float32)
    rw = np.asarray(inputs["rec_w_in"][0], np.float32)
    val = {
        "att_w": f(aw), "att_w_sw": f(aw[:, _pairswap(aw.shape[1])]), "att_wo": f(inputs["att_w_out"][0]),
        "rec_w": f(rw), "rec_w_sw": f(rw[:, _pairswap(rw.shape[1])]), "rec_wo": f(inputs["rec_w_out"][0]),
        "dlam": f(np.broadcast_to(np.asarray(inputs["diff_lambda"][0], np.float32)[None], (128, 4, 64))),
    }
    rope = np.zeros((6, 128, NLAT), np.float32)
    t = np.arange(NLAT)
    row = (t // 64).astype(np.float32)
    colp = (t % 64).astype(np.float32)
    for idx, hd in ((0, 128), (2, 64), (4, 64)):
        axis_dim = hd // 2
        inv_freq = (np.float32(10000.0) ** (-np.arange(0, axis_dim, 2, dtype=np.float32) / np.float32(axis_dim))).astype(np.float32)
        ang = np.concatenate([row[:, None] * inv_freq[None, :], colp[:, None] * inv_freq[None, :]], axis=-1).astype(np.float32)
        c = np.cos(ang).astype(np.float32)
        sn = np.sin(ang).astype(np.float32)
        C = np.repeat(c, 2, axis=1).T
        S = np.repeat(sn, 2, axis=1).T
        S[0::2, :] *= -1.0
        reps = 128 // hd
        rope[idx] = np.tile(C, (reps, 1))
        rope[idx + 1] = np.tile(S, (reps, 1))
    val["rope"] = rope
    s_i = np.arange(64)[:, None]
    c_i = np.arange(64)[None, :]
    masks = np.stack([(s_i <= c_i), (s_i >= c_i)], axis=1).astype(np.float32)
    val["masks"] = f(masks)
    rtab = np.zeros((128, 24, 64), np.float32)
    rdec = np.zeros((128, 4), np.float32)
    p = np.arange(64, dtype=np.float64)
    for gi in range(4):
        lg = np.log(np.float64(np.float32(1.0) - np.float32(2.0) ** np.float32(-5.0 - gi)))
        rdec[:, gi] = np.exp(64 * lg)
        for d in range(2):
            bb = (p + 1) * lg if d == 0 else (64 - p) * lg
            o = (gi * 2 + d) * 3
            rtab[:, o + 0, :] = np.exp(bb)[None, :]
            rtab[:, o + 1, :] = np.exp(-bb)[None, :]
            rtab[:, o + 2, :] = np.exp(64 * lg - bb)[None, :]
    val["rtab"] = rtab
    val["rdec"] = rdec
    _SHARED["key"] = key
    _SHARED["val"] = val
    return val


_CACHE = {}


def kernel(**inputs):
    cfg = {"stages": ("mixer", "moe")}
    if "nc" not in _CACHE:
        _CACHE["nc"] = build(cfg)
    nc = _CACHE["nc"]
    n = 8
    in_maps = [host_inputs(inputs, b) for b in range(n)]
    res = run_bass_kernel_spmd(nc, in_maps, core_ids=list(range(n)))
    return np.stack([r["out"] for r in res.results], axis=0).astype(np.float32)
```

```python
import contextlib
import math
import numpy as np
import concourse.bass as bass
import concourse.mybir as mybir
from concourse.bass_utils import run_bass_kernel_spmd

F32 = mybir.dt.float32
BF16 = mybir.dt.bfloat16
AF = mybir.ActivationFunctionType
ALU = mybir.AluOpType
AX = mybir.AxisListType


class Res:
    __slots__ = ("name", "w", "r")

    def __init__(self, name=""):
        self.name = name
        self.w = None
        self.r = []


class _Op:
    __slots__ = ("eng", "fn", "deps", "idx", "sig", "dma", "sem", "val", "nsig")


class Prog:
    COMPUTE = ("pe", "act", "dve", "pool")
    NDMASEM = 8

    def __init__(self):
        self.ops = []
        self.dma_count = {"sp": 0, "act": 0, "pool": 0}
        self.last = {}
        self.dma_recent = {"sp": [], "act": [], "pool": []}

    def add(self, eng, fn, reads=(), writes=(), dma=False, extra_deps=()):
        op = _Op()
        op.eng, op.fn, op.dma = eng, fn, dma
        op.idx = len(self.ops)
        op.sig = None
        op.nsig = False
        deps = set(extra_deps)
        for r in reads:
            if r.w is not None:
                deps.add(r.w)
        for w in writes:
            if w.w is not None:
                deps.add(w.w)
            deps.update(w.r)
        for r in reads:
            r.r.append(op.idx)
        for w in writes:
            w.w = op.idx
            w.r = []
        if dma:
            j = self.dma_count[eng]
            self.dma_count[eng] = j + 1
            op.sem = (eng, j % self.NDMASEM)
            op.val = 16 * (j // self.NDMASEM + 1)
            rec = self.dma_recent[eng]
            rec.append(op.idx)
            if len(rec) > self.NDMASEM:
                rec.pop(0)
        elif fn is not None:
            self.last[eng] = op.idx
        if eng == "pe" and not dma:
            deps = {d for d in deps if not (self.ops[d].eng == "pe" and not self.ops[d].dma)}
        op.deps = deps
        self.ops.append(op)
        return op.idx

    def pe(self, fn, reads=(), writes=()):
        return self.add("pe", fn, reads, writes)

    def act(self, fn, reads=(), writes=()):
        return self.add("act", fn, reads, writes)

    def dve(self, fn, reads=(), writes=()):
        return self.add("dve", fn, reads, writes)

    def pool(self, fn, reads=(), writes=()):
        return self.add("pool", fn, reads, writes)

    def dma(self, q, fn, reads=(), writes=()):
        return self.add(q, fn, reads, writes, dma=True)

    def barrier(self):
        deps = set(self.last.values())
        for q in self.dma_recent:
            deps.update(self.dma_recent[q])
        for e in ("pe", "act", "dve", "pool", "sp"):
            self.add(e, None, extra_deps=deps)

    def emit(self, nc, final_wait_ops=()):
        ops = self.ops
        for op in ops:
            for d in op.deps:
                if not ops[d].dma:
                    ops[d].nsig = True
        for i in final_wait_ops:
            if not ops[i].dma:
                ops[i].nsig = True
        cnt = {e: 0 for e in self.COMPUTE}
        for op in ops:
            if not op.dma and op.nsig:
                cnt[op.eng] += 1
                op.sig = cnt[op.eng]
        engobj = {"pe": nc.tensor, "act": nc.scalar, "dve": nc.vector, "pool": nc.gpsimd, "sp": nc.sync}
        with contextlib.ExitStack() as st:
            esem = {e: st.enter_context(nc.semaphore("s_" + e)) for e in self.COMPUTE}
            dsem = {}
            for q in ("sp", "act", "pool"):
                if self.dma_count[q]:
                    for k in range(self.NDMASEM):
                        dsem[(q, k)] = st.enter_context(nc.semaphore("d_%s%d" % (q, k)))
            block = st.enter_context(nc.Block())
            per_eng = {e: [] for e in engobj}
            for op in ops:
                per_eng[op.eng].append(op)

            def gen(ename):
                eng = engobj[ename]
                waited = {}

                def need(key, semh, val):
                    if waited.get(key, 0) >= val:
                        return
                    waited[key] = val
                    eng.wait_ge(semh, val)

                for op in per_eng[ename]:
                    for d in sorted(op.deps):
                        dop = ops[d]
                        if dop.dma:
                            need(dop.sem, dsem[dop.sem], dop.val)
                        else:
                            need(dop.eng, esem[dop.eng], dop.sig)
                    if op.fn is None:
                        continue
                    if op.dma and op.val > 16:
                        need(op.sem, dsem[op.sem], op.val - 16)
                    ins = op.fn(eng)
                    if op.dma:
                        ins.then_inc(dsem[op.sem], 16)
                    elif op.sig is not None:
                        ins.then_inc(esem[op.eng], 1)
                if ename == "sp":
                    for i in final_wait_ops:
                        fop = ops[i]
                        if fop.dma:
                            need(fop.sem, dsem[fop.sem], fop.val)
                        else:
                            need(fop.eng, esem[fop.eng], fop.sig)

            @block.sync
            def _(e):
                gen("sp")

            @block.tensor
            def _(e):
                gen("pe")

            @block.scalar
            def _(e):
                gen("act")

            @block.vector
            def _(e):
                gen("dve")

            @block.gpsimd
            def _(e):
                gen("pool")


D = 1024
KC = 8
NCTX = 256
NLAT = 2048
T = NCTX + NLAT
NE = 32
DEPTH = 2
CH = [(0, 256), (256, 512), (768, 512), (1280, 512), (1792, 512)]
EPS = 1e-6
LIMIT = 7.0
ALPHA = 1.702

V_BADA = 0
V_NMIX = V_BADA + DEPTH * 48
V_NFFN = V_NMIX + DEPTH * 8
V_BGU = V_NFFN + DEPTH * 8
V_ATT = V_BGU + DEPTH * NE * 16
V_REC = V_ATT + 9
NVEC = V_REC + 26


_UNIQ = [0]


def _sb(nc, name, shape, dtype):
    _UNIQ[0] += 1
    return nc.sbuf_tensor("%s_u%d" % (name, _UNIQ[0]), shape, dtype)


class K:
    pass


def mm(P, out, lhsT, rhs, start, stop, reads, writes):
    return P.pe(lambda e: e.matmul(out, lhsT=lhsT, rhs=rhs, start=start, stop=stop), reads, writes)


def tr(P, out, in_, ident, reads, writes):
    return P.pe(lambda e: e.transpose(out, in_, ident), reads, writes)


def act(P, out, in_, func, reads, writes, bias=0.0, scale=1.0, accum_out=None, eng="act"):
    if accum_out is None:
        return P.add(eng, lambda e: e.activation(out=out, in_=in_, func=func, bias=bias, scale=scale), reads, writes)
    return P.add(eng, lambda e: e.activation(out=out, in_=in_, func=func, bias=bias, scale=scale, accum_out=accum_out), reads, writes)


def ts(P, eng, out, in0, s1, s2, op0, op1, reads, writes, accum_out=None):
    if op1 is None:
        return P.add(eng, lambda e: e.tensor_scalar(out=out, in0=in0, scalar1=s1, scalar2=None, op0=op0), reads, writes)
    if accum_out is not None:
        return P.add(eng, lambda e: e.tensor_scalar(out=out, in0=in0, scalar1=s1, scalar2=s2, op0=op0, op1=op1, accum_out=accum_out), reads, writes)
    return P.add(eng, lambda e: e.tensor_scalar(out=out, in0=in0, scalar1=s1, scalar2=s2, op0=op0, op1=op1), reads, writes)


def tt(P, eng, out, in0, in1, op, reads, writes):
    return P.add(eng, lambda e: e.tensor_tensor(out=out, in0=in0, in1=in1, op=op), reads, writes)


def stt(P, out, in0, scalar, in1, op0, op1, reads, writes, accum_out=None):
    if accum_out is None:
        return P.dve(lambda e: e.scalar_tensor_tensor(out=out, in0=in0, scalar=scalar, in1=in1, op0=op0, op1=op1), reads, writes)
    return P.dve(lambda e: e.scalar_tensor_tensor(out=out, in0=in0, scalar=scalar, in1=in1, op0=op0, op1=op1, accum_out=accum_out), reads, writes)


def cp(P, eng, out, in_, reads, writes):
    return P.add(eng, lambda e: e.tensor_copy(out=out, in_=in_), reads, writes)


def dma(P, q, out, in_, reads, writes):
    return P.dma(q, lambda e: e.dma_start(out=out, in_=in_), reads, writes)


def getps(k):
    i = k.ps_next % k.ps_n
    k.ps_next = (i + 1) % k.ps_n
    return k.ps[i], k.psr[i]


def load_tokens(k, st_ext=None):
    P, nc = k.P, k.nc
    with contextlib.ExitStack() as st_own:
        st = st_ext if st_ext is not None else st_own
        stage = [st.enter_context(_sb(nc, "ldst%d" % i, [128, D], F32)) for i in range(3)]
        sres = [Res() for _ in range(3)]
        for ti in range(T // 128):
            s = ti % 3
            src = k.ctx_d[ti * 128:(ti + 1) * 128, :] if ti < 2 else k.x_d[(ti - 2) * 128:(ti - 1) * 128, :]
            dma(P, "sp", stage[s][:], src, [], [sres[s]])
            ci, off = chunk_of(ti * 128)
            for half in range(2):
                ps, pr = getps(k)
                for j in range(4):
                    kc = half * 4 + j
                    tr(P, ps[:, j * 128:(j + 1) * 128], stage[s][:, kc * 128:(kc + 1) * 128], k.identf[:], [sres[s], k.cres], [pr])
                eng = "dve" if half == 0 else "act"
                dst = k.hT[:, half * 4:half * 4 + 4, ti * 128:(ti + 1) * 128]
                src_ps = ps[:, :].rearrange("p (j t) -> p j t", j=4)
                wr = [k.hres[half * 4 + j][ci] for j in range(4)]
                if eng == "dve":
                    cp(P, "dve", dst, src_ps, [pr], wr)
                else:
                    act(P, dst, src_ps, AF.Copy, [pr], wr)
        if st_ext is None:
            P.barrier()


def chunk_of(t):
    for ci, (t0, tn) in enumerate(CH):
        if t0 <= t < t0 + tn:
            return ci, t - t0
    raise ValueError


def store_output(k):
    P, nc = k.P, k.nc
    outs = []
    with contextlib.ExitStack() as st:
        stage = [st.enter_context(_sb(nc, "stst%d" % i, [128, D], F32)) for i in range(3)]
        sres = [Res() for _ in range(3)]
        for ti in range(2, T // 128):
            s = ti % 3
            ci, off = chunk_of(ti * 128)
            for half in range(2):
                ps, pr = getps(k)
                for j in range(4):
                    kc = half * 4 + j
                    tr(P, ps[:, j * 128:(j + 1) * 128], k.hT[:, kc, ti * 128:(ti + 1) * 128], k.identf[:], [k.hres[kc][ci], k.cres], [pr])
                dst = stage[s][:, half * 512:(half + 1) * 512]
                if half == 0:
                    cp(P, "dve", dst, ps[:, :], [pr], [sres[s]])
                else:
                    act(P, dst, ps[:, :], AF.Copy, [pr], [sres[s]])
            outs.append(dma(P, "sp", k.out_d[(ti - 2) * 128:(ti - 1) * 128, :], stage[s][:], [sres[s]], []))
        P.barrier()
    return outs


def modulation(k):
    P, nc = k.P, k.nc
    with contextlib.ExitStack() as st:
        ccf = st.enter_context(_sb(nc, "ccf", [128, KC, 2], F32))
        sT = st.enter_context(_sb(nc, "sT", [128, KC, 2], BF16))
        wt = [st.enter_context(_sb(nc, "wada%d" % i, [128, KC, 768], BF16)) for i in range(2)]
        wres = [Res() for _ in range(2)]
        rcc, rs = Res(), Res()
        dma(P, "sp", ccf[:], k.cc_d[:, :, :], [], [rcc])
        act(P, sT[:], ccf[:], AF.Silu, [rcc], [rs])
        n = 0
        for l in range(DEPTH):
            ps, pr = getps(k)
            for g in range(8):
                s = n % 2
                n += 1
                src = k.wada_d[l].rearrange("(kc p) f -> p kc f", p=128)[:, :, g * 768:(g + 1) * 768]
                dma(P, "pool", wt[s][:], src, [], [wres[s]])
                for jj in range(6):
                    j = g * 6 + jj
                    for kc in range(KC):
                        mm(P, ps[:, 2 * j:2 * j + 2], wt[s][:, kc, jj * 128:(jj + 1) * 128], sT[:, kc, :],
                           kc == 0, kc == KC - 1, [wres[s], rs], [pr])
            for w in range(2):
                src_ps = ps[:, 0:96].rearrange("p (j w) -> p j w", w=2)[:, :, w]
                tt(P, "dve", k.mod[l][:, w, :], src_ps, k.vecs[:, V_BADA + l * 48:V_BADA + (l + 1) * 48], ALU.add,
                   [pr, k.cres], [k.modres])
                stt(P, k.A1[l][:, w, :], k.mod[l][:, w, 8:16], 1.0, k.vecs[:, V_NMIX + l * 8:V_NMIX + l * 8 + 8], ALU.add, ALU.mult,
                    [k.modres, k.cres], [k.modres])
                stt(P, k.A2[l][:, w, :], k.mod[l][:, w, 32:40], 1.0, k.vecs[:, V_NFFN + l * 8:V_NFFN + l * 8 + 8], ALU.add, ALU.mult,
                    [k.modres, k.cres], [k.modres])
        P.barrier()


def norm_mod(k, l, which, f32cb=None, skip_ctx=False):
    P, nc = k.P, k.nc
    A = k.A1[l] if which == 0 else k.A2[l]
    shj = 0 if which == 0 else 24
    with contextlib.ExitStack() as st:
        sq = [st.enter_context(_sb(nc, "nsq%d" % i, [128, 512], F32)) for i in range(2)]
        sqr = [Res() for _ in range(2)]
        rstd = [st.enter_context(_sb(nc, "nrs%d" % i, [128, 512], F32)) for i in range(2)]
        rsr = [Res() for _ in range(2)]
        tmp = [st.enter_context(_sb(nc, "ntm%d" % i, [128, 512], F32)) for i in range(2)]
        tmr = [Res() for _ in range(2)]
        if f32cb is not None:
            f32t = [st.enter_context(_sb(nc, "nf32%d" % i, [128, KC, 512], F32)) for i in range(2)]
            f32r = [Res() for _ in range(2)]
        cnt = {"nsq": 0, "ntm": 0}
        pss = {}

        def stage1(ci):
            t0, tn = CH[ci]
            ps, pr = getps(k)
            for kc in range(KC):
                s = cnt["nsq"] % 2
                cnt["nsq"] += 1
                act(P, sq[s][:, :tn], k.hT[:, kc, t0:t0 + tn], AF.Square, [k.hres[kc][ci]], [sqr[s]])
                mm(P, ps[:, :tn], k.onesf[:], sq[s][:, :tn], kc == 0, kc == KC - 1, [sqr[s], k.cres], [pr])
            r = ci % 2
            act(P, rstd[r][:, :tn], ps[:, :tn], AF.Ln, [pr], [rsr[r]], bias=k.epsc[:, 0:1], scale=1.0 / D)
            act(P, rstd[r][:, :tn], rstd[r][:, :tn], AF.Exp, [rsr[r]], [rsr[r]], scale=-0.5)

        def stage2(ci):
            t0, tn = CH[ci]
            w = 1 if ci == 0 else 0
            r = ci % 2
            for kc in range(KC):
                s = cnt["ntm"] % 2
                cnt["ntm"] += 1
                stt(P, tmp[s][:, :tn], k.hT[:, kc, t0:t0 + tn], A[:, w, kc:kc + 1], rstd[r][:, :tn], ALU.mult, ALU.mult,
                    [k.hres[kc][ci], k.modres, rsr[r]], [tmr[s]])
                shc = k.mod[l][:, w, shj + kc:shj + kc + 1]
                if f32cb is None:
                    act(P, k.xT[:, kc, t0:t0 + tn], tmp[s][:, :tn], AF.Identity, [tmr[s], k.modres], [k.xres[kc][ci]], bias=shc)
                else:
                    fr = ci % 2
                    act(P, f32t[fr][:, kc, :tn], tmp[s][:, :tn], AF.Identity, [tmr[s], k.modres], [f32r[fr]], bias=shc)
                    cp(P, "pool", k.xT[:, kc, t0:t0 + tn], f32t[fr][:, kc, :tn], [f32r[fr]], [k.xres[kc][ci]])
            if f32cb is not None:
                f32cb(ci, t0, tn, f32t[ci % 2], f32r[ci % 2])

        cis = [ci for ci in range(len(CH)) if not (skip_ctx and ci == 0)]
        stage1(cis[0])
        for i_, ci in enumerate(cis):
            if i_ + 1 < len(cis):
                stage1(cis[i_ + 1])
            stage2(ci)
        P.barrier()


def moe_layer(k, l):
    P, nc = k.P, k.nc
    with contextlib.ExitStack() as st:
        last = (l == DEPTH - 1)
        GT = st.enter_context(_sb(nc, "GT", [NE, T], F32))
        gtres = [Res() for _ in CH]

        with contextlib.ExitStack() as st2:
            wr_t = st2.enter_context(_sb(nc, "wrt", [128, KC, NE], F32))
            br_t = st2.enter_context(_sb(nc, "brt", [1, NE], F32))
            bd_t = st2.enter_context(_sb(nc, "bdt", [NE, D], F32))
            wres = Res()
            dma(P, "sp", wr_t[:], k.wrouter_d[l].rearrange("(kc p) e -> p kc e", p=128), [], [wres])
            dma(P, "sp", br_t[:], k.brouter_d[l:l + 1, :], [], [wres])
            dma(P, "sp", bd_t[:], k.bdown_d[l], [], [wres])
            lg = [st2.enter_context(_sb(nc, "lg%d" % i, [128, NE], F32)) for i in range(2)]
            ex = [st2.enter_context(_sb(nc, "ex%d" % i, [128, NE], F32)) for i in range(2)]
            msk = [st2.enter_context(_sb(nc, "mk%d" % i, [128, NE], F32)) for i in range(2)]
            t8 = [st2.enter_context(_sb(nc, "t8%d" % i, [128, 8], F32)) for i in range(2)]
            den = [st2.enter_context(_sb(nc, "dn%d" % i, [128, 2], F32)) for i in range(2)]
            rr = [Res() for _ in range(2)]
            cnt = [0]

            def router_cb(ci, t0, tn, f32tile, f32res):
                for sub in range(tn // 128):
                    s = cnt[0] % 2
                    cnt[0] += 1
                    ps, pr = getps(k)
                    for kc in range(KC):
                        mm(P, ps[:, 0:NE], f32tile[:, kc, sub * 128:(sub + 1) * 128], wr_t[:, kc, :], kc == 0, False,
                           [f32res, wres], [pr])
                    mm(P, ps[:, 0:NE], k.onesf[0:1, :], br_t[0:1, :], False, True, [wres, k.cres], [pr])
                    cp(P, "dve", lg[s][:], ps[:, 0:NE], [pr], [rr[s]])
                    P.dve(lambda e, o=t8[s][:], i=lg[s][:]: e.max(out=o, in_=i), [rr[s]], [rr[s]])
                    ts(P, "dve", msk[s][:], lg[s][:], t8[s][:, 3:4], None, ALU.is_ge, None, [rr[s]], [rr[s]])
                    ts(P, "dve", den[s][:, 1:2], t8[s][:, 0:1], -1.0, None, ALU.mult, None, [rr[s]], [rr[s]])
                    act(P, ex[s][:], lg[s][:], AF.Exp, [rr[s]], [rr[s]], bias=den[s][:, 1:2])
                    stt(P, ex[s][:], ex[s][:], 1.0, msk[s][:], ALU.mult, ALU.mult, [rr[s]], [rr[s]], accum_out=den[s][:, 0:1])
                    P.dve(lambda e, o=den[s][:, 0:1]: e.reciprocal(out=o, in_=o), [rr[s]], [rr[s]])
                    ts(P, "dve", ex[s][:], ex[s][:], den[s][:, 0:1], None, ALU.mult, None, [rr[s]], [rr[s]])
                    ps2, pr2 = getps(k)
                    tr(P, ps2[0:NE, 0:128], ex[s][:], k.identf[:], [rr[s], k.cres], [pr2])
                    cp(P, "dve", GT[:, t0 + sub * 128:t0 + (sub + 1) * 128], ps2[0:NE, 0:128], [pr2], [gtres[ci]])

            norm_mod(k, l, 1, f32cb=router_cb, skip_ctx=last)

            for ci, (t0, tn) in enumerate(CH):
                if last and ci == 0:
                    continue
                w = 1 if ci == 0 else 0
                for dc in range(KC):
                    ps, pr = getps(k)
                    mm(P, ps[:, :tn], bd_t[:, dc * 128:(dc + 1) * 128], GT[:, t0:t0 + tn], True, True, [wres, gtres[ci]], [pr])
                    stt(P, k.hT[:, dc, t0:t0 + tn], ps[:, :tn], k.mod[l][:, w, 40 + dc:41 + dc], k.hT[:, dc, t0:t0 + tn], ALU.mult, ALU.add,
                        [pr, k.modres, k.hres[dc][ci]], [k.hres[dc][ci]])
            P.barrier()

        NSLOT = 8
        PW = 256
        GW = 768
        ring = [st.enter_context(_sb(nc, "wring%d" % i, [128, KC, PW], BF16)) for i in range(NSLOT)]
        ringres = [Res() for _ in range(NSLOT)]
        if last:
            groups = [[(256, 512, 1), (768, 256, 2)], [(1024, 256, 2), (1280, 512, 3)], [(1792, 512, 4)]]
        else:
            groups = [[(0, 256, 0), (256, 512, 1)], [(768, 512, 2), (1280, 256, 3)], [(1536, 256, 3), (1792, 512, 4)]]
        hm = [st.enter_context(_sb(nc, "hmid%d" % i, [128, KC, GW], BF16)) for i in range(2)]
        hmres = [[[Res() for _ in range(2)] for _ in range(KC)] for _ in range(2)]
        gb = [st.enter_context(_sb(nc, "gb%d" % i, [128, GW], F32)) for i in range(2)]
        gbres = [[Res() for _ in range(2)] for _ in range(2)]
        NT = 4
        SKEW = 1
        gc = [st.enter_context(_sb(nc, "gc%d" % i, [128, 512], F32)) for i in range(NT)]
        u0 = [st.enter_context(_sb(nc, "u0%d" % i, [128, 512], F32)) for i in range(NT)]
        tres = [Res() for _ in range(NT)]

        items = [(gi, e) for gi in range(len(groups)) for e in range(NE)]
        pieces = []

        def gu_pieces(i):
            for q in range(4):
                pieces.append((i, "g", q))
                pieces.append((i, "u", q))

        def d_pieces(i):
            for r in range(4):
                pieces.append((i, "d", r))

        gu_pieces(0)
        for i in range(len(items)):
            if i + 1 < len(items):
                gu_pieces(i + 1)
            d_pieces(i)
        state = {"issued": 0, "used": 0}
        slot_of = {}
        PRE = NSLOT - 2

        def issue_upto(n):
            while state["issued"] < min(n, len(pieces)):
                i = state["issued"]
                it, kind, q = pieces[i]
                e = items[it][1]
                s_ = i % NSLOT
                slot_of[pieces[i]] = s_
                if kind == "g":
                    src = k.wgu_d[l, e].rearrange("(kc p) f -> p kc f", p=128)[:, :, q * PW:(q + 1) * PW]
                elif kind == "u":
                    src = k.wgu_d[l, e].rearrange("(kc p) f -> p kc f", p=128)[:, :, 1024 + q * PW:1024 + (q + 1) * PW]
                else:
                    src = k.wdown_d[l, e].rearrange("(kc p) f -> p kc f", p=128)[:, :, q * PW:(q + 1) * PW]
                dma(P, "pool", ring[s_][:], src, [], [ringres[s_]])
                state["issued"] += 1

        def take(key):
            issue_upto(state["used"] + PRE)
            state["used"] += 1
            return slot_of[key]

        nt = [0]
        pend_bc = []

        def GU(i):
            gi, e = items[i]
            grp = groups[gi]
            gt0 = grp[0][0]
            hb = i % 2
            for li, (t0, tn, ci) in enumerate(grp):
                ps, pr = getps(k)
                mm(P, ps[:, :tn], k.identf[0:NE, e:e + 1].broadcast_to([NE, 128]), GT[:, t0:t0 + tn], True, True, [k.cres, gtres[ci]], [pr])
                act(P, gb[hb][:, t0 - gt0:t0 - gt0 + tn], ps[:, :tn], AF.Copy, [pr], [gbres[hb][li]], scale=1.0 / ALPHA)
            for q in range(4):
                sg_ = take((i, "g", q))
                su_ = take((i, "u", q))
                for j in range(2):
                    fc = 2 * q + j
                    vb = V_BGU + (l * NE + e) * 16
                    bgc = k.vecs[:, vb + fc:vb + fc + 1]
                    buc = k.vecs[:, vb + 8 + fc:vb + 8 + fc + 1]
                    for li, (t0, tn, ci) in enumerate(grp):
                        lo = t0 - gt0
                        psg, prg = getps(k)
                        for kc in range(KC):
                            mm(P, psg[:, :tn], ring[sg_][:, kc, j * 128:(j + 1) * 128], k.xT[:, kc, t0:t0 + tn], kc == 0, kc == KC - 1,
                               [ringres[sg_], k.xres[kc][ci]], [prg])
                        psu, pru = getps(k)
                        for kc in range(KC):
                            mm(P, psu[:, :tn], ring[su_][:, kc, j * 128:(j + 1) * 128], k.xT[:, kc, t0:t0 + tn], kc == 0, kc == KC - 1,
                               [ringres[su_], k.xres[kc][ci]], [pru])
                        s_ = nt[0] % NT
                        nt[0] += 1
                        R = [tres[s_]]
                        ts(P, "dve", gc[s_][:, :tn], psg[:, :tn], bgc, LIMIT, ALU.add, ALU.min, [prg, k.cres], R)
                        act(P, gc[s_][:, :tn], gc[s_][:, :tn], AF.Silu, R, R, scale=ALPHA)
                        act(P, u0[s_][:, :tn], psu[:, :tn], AF.Identity, [pru, k.cres], R, bias=buc)

                        def stage_bc(s_=s_, tn=tn, lo=lo, hb=hb, li=li, fc=fc, R=R):
                            ts(P, "pool", u0[s_][:, :tn], u0[s_][:, :tn], LIMIT, -LIMIT, ALU.min, ALU.max, R, R)
                            tt(P, "dve", gc[s_][:, :tn], gc[s_][:, :tn], gb[hb][:, lo:lo + tn], ALU.mult, R + [gbres[hb][li]], R)
                            stt(P, hm[hb][:, fc, lo:lo + tn], u0[s_][:, :tn], 1.0, gc[s_][:, :tn], ALU.add, ALU.mult, R, [hmres[hb][fc][li]])

                        if len(pend_bc) >= SKEW:
                            pend_bc.pop(0)()
                        pend_bc.append(stage_bc)

        def DOWN(i):
            gi, e = items[i]
            grp = groups[gi]
            gt0 = grp[0][0]
            hb = i % 2
            for r in range(4):
                sd_ = take((i, "d", r))
                for j in range(2):
                    dc = 2 * r + j
                    for li, (t0, tn, ci) in enumerate(grp):
                        lo = t0 - gt0
                        w = 1 if ci == 0 else 0
                        ps, pr = getps(k)
                        for fc in range(KC):
                            mm(P, ps[:, :tn], ring[sd_][:, fc, j * 128:(j + 1) * 128], hm[hb][:, fc, lo:lo + tn], fc == 0, fc == KC - 1,
                               [ringres[sd_], hmres[hb][fc][li]], [pr])
                        stt(P, k.hT[:, dc, t0:t0 + tn], ps[:, :tn], k.mod[l][:, w, 40 + dc:41 + dc], k.hT[:, dc, t0:t0 + tn],
                            ALU.mult, ALU.add, [pr, k.modres, k.hres[dc][ci]], [k.hres[dc][ci]])

        GU(0)
        for i in range(len(items)):
            if i + 1 < len(items):
                GU(i + 1)
            else:
                while pend_bc:
                    pend_bc.pop(0)()
            DOWN(i)
        P.barrier()


def wload(k, dst, Wd, c0, n, res):
    src = Wd.rearrange("(kc p) f -> p kc f", p=128)[:, :, c0:c0 + n]
    return dma(k.P, "pool", dst, src, [], [res])


def fm_proj(k, ps_ap, pr, wt, wres, c0, n, ci, t0, tn):
    for kc in range(KC):
        mm(k.P, ps_ap, wt[:, kc, c0:c0 + n], k.xT[:, kc, t0:t0 + tn], kc == 0, kc == KC - 1, [wres, k.xres[kc][ci]], [pr])


def rstd_from_ps(k, dst, ps_ap, pr, dres, n_inv):
    P = k.P
    npart = dst.shape[0]
    act(P, dst, ps_ap, AF.Ln, [pr], [dres], bias=k.epsc[0:npart, 0:1], scale=n_inv)
    act(P, dst, dst, AF.Exp, [dres], [dres], scale=-0.5)


def qk_prep(k, name, Wd, Wswd, c0, nf, gcol, gscol, ones_ap, hd, rope_i, dst, dres_fn, with_ctx, post_scale=1.0, norm=True, dbuf=True):
    P, nc = k.P, k.nc
    with contextlib.ExitStack() as st:
        wt = st.enter_context(_sb(nc, name + "w", [128, KC, nf], BF16))
        wts = st.enter_context(_sb(nc, name + "ws", [128, KC, nf], BF16))
        wres = Res()
        wload(k, wt[:], Wd, c0, nf, wres)
        wload(k, wts[:], Wswd, c0, nf, wres)
        tl = [[st.enter_context(_sb(nc, name + "t%d%d" % (i, j), [nf, 512], F32)) for j in range(4)] for i in range(2 if dbuf else 1)] * (1 if dbuf else 2)
        tr_ = [Res(), Res()] if dbuf else [Res()] * 2
        rpc = [st.enter_context(_sb(nc, name + "rc%d" % i, [nf, 512], F32)) for i in range(2)]
        rps = [st.enter_context(_sb(nc, name + "rs%d" % i, [nf, 512], F32)) for i in range(2)]
        rpr = [[Res(), Res()] for _ in range(2)]
        for n_, (ci, (t0, tn)) in enumerate([(ci, c) for ci, c in enumerate(CH) if (with_ctx or ci > 0)]):
            b = n_ % 2
            sq, rs, x1, x2 = tl[b]
            R = [tr_[b]]
            lat = ci > 0
            ps1, pr1 = getps(k)
            fm_proj(k, ps1[0:nf, :tn], pr1, wt, wres, 0, nf, ci, t0, tn)
            if norm:
                act(P, sq[:, :tn], ps1[0:nf, :tn], AF.Square, [pr1], R)
                pss, prs = getps(k)
                mm(P, pss[0:nf, :tn], ones_ap, sq[:, :tn], True, True, R + [k.cres], [prs])
                rstd_from_ps(k, rs[:, :tn], pss[0:nf, :tn], prs, tr_[b], 1.0 / hd)
                stt(P, x1[:, :tn], ps1[0:nf, :tn], gcol, rs[:, :tn], ALU.mult, ALU.mult, [pr1, k.cres] + R, R)
            else:
                act(P, x1[:, :tn], ps1[0:nf, :tn], AF.Copy, [pr1], R, scale=post_scale)
            dcol = t0 if with_ctx else t0 - NCTX
            if lat:
                ps2, pr2 = getps(k)
                fm_proj(k, ps2[0:nf, :tn], pr2, wts, wres, 0, nf, ci, t0, tn)
                if norm:
                    stt(P, x2[:, :tn], ps2[0:nf, :tn], gscol, rs[:, :tn], ALU.mult, ALU.mult, [pr2, k.cres] + R, R)
                else:
                    act(P, x2[:, :tn], ps2[0:nf, :tn], AF.Copy, [pr2], R, scale=post_scale)
                pb = n_ % 2
                dma(P, "sp", rpc[pb][:, :tn], k.rope_d[rope_i, 0:nf, t0 - NCTX:t0 - NCTX + tn], [], [rpr[pb][0]])
                dma(P, "sp", rps[pb][:, :tn], k.rope_d[rope_i + 1, 0:nf, t0 - NCTX:t0 - NCTX + tn], [], [rpr[pb][1]])
                tt(P, "dve", x1[:, :tn], x1[:, :tn], rpc[pb][:, :tn], ALU.mult, R + [rpr[pb][0]], R)
                tt(P, "dve", x2[:, :tn], x2[:, :tn], rps[pb][:, :tn], ALU.mult, R + [rpr[pb][1]], R)
                tt(P, "dve", dst[:, dcol:dcol + tn], x1[:, :tn], x2[:, :tn], ALU.add, R, [dres_fn(ci)])
            else:
                cp(P, "dve", dst[:, dcol:dcol + tn], x1[:, :tn], R, [dres_fn(ci)])
        P.barrier()


def v_prep(k, name, Wd, c0, nv, dst, dres, rows=128):
    P, nc = k.P, k.nc
    with contextlib.ExitStack() as st:
        wt = st.enter_context(_sb(nc, name + "w", [128, KC, nv], BF16))
        wres = Res()
        wload(k, wt[:], Wd, c0, nv, wres)
        for ti in range(T // rows):
            ci, _ = chunk_of(ti * rows)
            ps, pr = getps(k)
            for kc in range(KC):
                mm(P, ps[0:rows, 0:nv], k.xT[:, kc, ti * rows:(ti + 1) * rows], wt[:, kc, :], kc == 0, kc == KC - 1,
                   [wres, k.xres[kc][ci]], [pr])
            if ti % 2 == 0:
                cp(P, "dve", dst[0:rows, ti, :], ps[0:rows, 0:nv], [pr], [dres])
            else:
                act(P, dst[0:rows, ti, :], ps[0:rows, 0:nv], AF.Copy, [pr], [dres])
        P.barrier()


def attn_core(k, KT_fn, QT_fn, V_fn, kdim_reads, scale, onesb, pt, ptres, qi, out_cb):
    P = k.P
    a = k.acc_next
    k.acc_next = (a + 1) % 2
    psO, prO = k.ps[4 + 2 * a], k.psr[4 + 2 * a]
    psL, prL = k.ps[5 + 2 * a], k.psr[5 + 2 * a]
    NTL = T // 128
    pend = []

    def issue_s(jt):
        psS, prS = getps(k)
        mm(P, psS[:, :], KT_fn(jt), QT_fn(qi), True, True, kdim_reads, [prS])
        s = k.pt_next
        k.pt_next = (s + 1) % len(pt)
        act(P, pt[s][:, :], psS[:, :], AF.Exp, [prS], [ptres[s]], scale=scale)
        pend.append((jt, s))

    def issue_o():
        jt, s = pend.pop(0)
        mm(P, psO[:, :], V_fn(jt), pt[s][:, :], jt == 0, jt == NTL - 1, [ptres[s]] + kdim_reads, [prO])
        mm(P, psL[:, :], onesb[:], pt[s][:, :], jt == 0, jt == NTL - 1, [ptres[s], k.cres], [prL])

    LOOK = 2
    for jt in range(NTL):
        issue_s(jt)
        if len(pend) > LOOK:
            issue_o()
    while pend:
        issue_o()
    out_cb(qi, psO, prO, psL, prL)


def out_proj_head(k, l, Wo_d, r0, nrows, y_ap_fn, yres_fn, chunk_list, name):
    P, nc = k.P, k.nc
    with contextlib.ExitStack() as st:
        wo = st.enter_context(_sb(nc, name + "wo", [nrows, D], BF16))
        wres = Res()
        dma(P, "pool", wo[:], Wo_d[r0:r0 + nrows, :], [], [wres])
        for dc in range(KC):
            for (ci, t0, tn, yc0) in chunk_list:
                w = 1 if ci == 0 else 0
                ps, pr = getps(k)
                mm(P, ps[:, :tn], wo[:, dc * 128:(dc + 1) * 128], y_ap_fn(yc0, tn), True, True, [wres, yres_fn(ci)], [pr])
                stt(P, k.hT[:, dc, t0:t0 + tn], ps[:, :tn], k.mod[l][:, w, 16 + dc:17 + dc], k.hT[:, dc, t0:t0 + tn], ALU.mult, ALU.add,
                    [pr, k.modres, k.hres[dc][ci]], [k.hres[dc][ci]])
        P.barrier()


def attention_mixer(k, l):
    P, nc = k.P, k.nc
    lam_init = 0.8 - 0.6 * math.exp(-0.3 * l)
    W, Wsw, Wo = k.attw_d, k.attwsw_d, k.attwo_d
    VA = V_ATT
    col = lambda i: k.vecs[:, VA + i:VA + i + 1]
    k.ps_n = 4
    k.ps_next = 0
    k.acc_next = 0
    k.pt_next = 0
    with contextlib.ExitStack() as st:
        onesb = st.enter_context(_sb(nc, "onesb", [128, 128], BF16))
        cp(P, "dve", onesb[:], k.onesf, [k.cres], [k.cres])
        lp = st.enter_context(_sb(nc, "lp", [128, 4, 64], F32))
        lpp = st.enter_context(_sb(nc, "lpp", [128, 2, 64], F32))
        lsm = st.enter_context(_sb(nc, "lsm", [128, 4], F32))
        dgc = st.enter_context(_sb(nc, "dgc", [128, 1], F32))
        lres = Res()
        dma(P, "sp", lp[:], k.dlam_d[:, :, :], [], [lres])
        tt(P, "dve", lpp[:], lp[:, 0::2, :], lp[:, 1::2, :], ALU.mult, [lres], [lres])
        P.dve(lambda e: e.reduce_sum(out=lsm[:, 0:2], in_=lpp[:], axis=AX.X), [lres], [lres])
        act(P, lsm[:, 0:2], lsm[:, 0:2], AF.Exp, [lres], [lres])
        tt(P, "dve", lsm[:, 2:3], lsm[:, 1:2], lsm[:, 0:1], ALU.subtract, [lres], [lres])
        ts(P, "dve", lsm[:, 3:4], lsm[:, 2:3], -lam_init, None, ALU.add, None, [lres], [lres])
        nlam = lsm[:, 3:4]
        ts(P, "dve", dgc[:], col(8), 1.0 - lam_init, None, ALU.mult, None, [k.cres], [lres])

        YT = st.enter_context(_sb(nc, "YT", [128, KC, NLAT], BF16))
        yres = [[Res() for _ in range(4)] for _ in range(KC)]
        pt = [st.enter_context(_sb(nc, "pt%d" % i, [128, 512], BF16)) for i in range(3)]
        ptres = [Res() for _ in range(3)]
        rl = [st.enter_context(_sb(nc, "rl%d" % i, [128, 512], F32)) for i in range(2)]
        rlres = [Res() for _ in range(2)]
        rln = [0]

        for kv in range(2):
            with contextlib.ExitStack() as s2:
                KT = s2.enter_context(_sb(nc, "gKT%d" % kv, [128, T], BF16))
                ktres = [Res() for _ in CH]
                Vt = s2.enter_context(_sb(nc, "gV%d" % kv, [128, T // 128, 128], BF16))
                vres = Res()
                QT = [s2.enter_context(_sb(nc, "gQT%d_%d" % (kv, i), [128, NLAT], BF16)) for i in range(2)]
                qres = [[Res() for _ in CH] for _ in range(2)]
                k.ps_n = 8
                qk_prep(k, "gk%d" % kv, W, Wsw, 512 + kv * 128, 128, col(2), col(3), k.onesf, 128, 0, KT, (lambda ci: ktres[ci]), True)
                v_prep(k, "gv%d" % kv, W, 768 + kv * 128, 128, Vt, vres)
                for g in range(2):
                    h = kv * 2 + g
                    qk_prep(k, "gq%d" % h, W, Wsw, h * 128, 128, col(0), col(1), k.onesf, 128, 0, QT[g], (lambda ci, g=g: qres[g][ci]), False)
                if kv == 0 and "KTd" in k.dbg:
                    k.dbg_ops.append(dma(P, "pool", k.dbg["KTd"], KT[:], ktres, []))
                    k.dbg_ops.append(dma(P, "pool", k.dbg["QTd"], QT[0][:], qres[0], []))
                    k.dbg_ops.append(dma(P, "pool", k.dbg["VTd"].rearrange("p (a b) -> p a b", b=128), Vt[:], [vres], []))
                    k.dbg_ops.append(dma(P, "pool", k.dbg["XTd"], k.xT[:, 0, :], [k.xres[0][ci] for ci in range(5)], []))
                k.ps_n = 4
                for g in range(2):
                    h = kv * 2 + g
                    for qi in range(4):
                        def cb(qi, psO, prO, psL, prL, h=h):
                            s = rln[0] % 2
                            rln[0] += 1
                            P.dve(lambda e, o=rl[s][:, :], i=psL[:, :]: e.reciprocal(out=o, in_=i), [prL], [rlres[s]])
                            tt(P, "dve", YT[:, h, qi * 512:(qi + 1) * 512], psO[:, :], rl[s][:, :], ALU.mult, [prO, rlres[s]], [yres[h][qi]])
                        attn_core(k, (lambda jt: KT[:, jt * 128:(jt + 1) * 128]), (lambda qi, g=g: QT[g][:, qi * 512:(qi + 1) * 512]),
                                  (lambda jt: Vt[:, jt, :]), ktres + qres[g] + [vres], 128 ** -0.5, onesb, pt, ptres, qi, cb)
                P.barrier()

        with contextlib.ExitStack() as s3:
            o0 = s3.enter_context(_sb(nc, "do0", [128, 512], F32))
            od = s3.enter_context(_sb(nc, "dod", [128, 512], F32))
            dsq = s3.enter_context(_sb(nc, "dsq", [128, 512], F32))
            ores = Res()
            for h in range(4):
                with contextlib.ExitStack() as s2:
                    KT = s2.enter_context(_sb(nc, "dKT%d" % h, [128, T], BF16))
                    ktres = [Res() for _ in CH]
                    Vt = s2.enter_context(_sb(nc, "dV%d" % h, [128, T // 128, 128], BF16))
                    vres = Res()
                    QT = s2.enter_context(_sb(nc, "dQT%d" % h, [128, NLAT], BF16))
                    qres = [Res() for _ in CH]
                    k.ps_n = 8
                    qk_prep(k, "dk%d" % h, W, Wsw, 1536 + h * 128, 128, col(6), col(7), k.ones2, 64, 2, KT, (lambda ci: ktres[ci]), True)
                    v_prep(k, "dv%d" % h, W, 2048 + h * 128, 128, Vt, vres)
                    qk_prep(k, "dq%d" % h, W, Wsw, 1024 + h * 128, 128, col(4), col(5), k.ones2, 64, 2, QT, (lambda ci: qres[ci]), False)
                    k.ps_n = 4
                    for qi in range(4):
                        for half in range(2):
                            def cb(qi, psO, prO, psL, prL, h=h, half=half):
                                s = rln[0] % 2
                                rln[0] += 1
                                P.dve(lambda e, o=rl[s][:, :], i=psL[:, :]: e.reciprocal(out=o, in_=i), [prL], [rlres[s]])
                                if half == 0:
                                    tt(P, "dve", o0[:, :], psO[:, :], rl[s][:, :], ALU.mult, [prO, rlres[s]], [ores])
                                    return
                                tt(P, "dve", od[:, :], psO[:, :], rl[s][:, :], ALU.mult, [prO, rlres[s], ores], [ores])
                                stt(P, od[:, :], od[:, :], nlam, o0[:, :], ALU.mult, ALU.add, [ores, lres], [ores])
                                act(P, dsq[:, :], od[:, :], AF.Square, [ores], [ores])
                                pss, prs = getps(k)
                                mm(P, pss[:, :], k.onesf, dsq[:, :], True, True, [ores, k.cres], [prs])
                                rstd_from_ps(k, dsq[:, :], pss[:, :], prs, ores, 1.0 / 128)
                                stt(P, YT[:, 4 + h, qi * 512:(qi + 1) * 512], od[:, :], dgc[:, 0:1], dsq[:, :], ALU.mult, ALU.mult,
                                    [ores, lres], [yres[4 + h][qi]])
                            p0 = half * 64
                            attn_core(k, (lambda jt, p0=p0: KT[p0:p0 + 64, jt * 128:(jt + 1) * 128]),
                                      (lambda qi, p0=p0: QT[p0:p0 + 64, qi * 512:(qi + 1) * 512]),
                                      (lambda jt: Vt[:, jt, :]), ktres + qres + [vres], 64 ** -0.5, onesb, pt, ptres, qi, cb)
                    P.barrier()

        if "YTd" in k.dbg:
            k.dbg_ops.append(dma(P, "pool", k.dbg["YTd"].rearrange("p (a b) -> p a b", a=KC), YT[:], [r for rr_ in yres for r in rr_], []))
        k.ps_n = 8
        chunk_list = [(ci, CH[ci][0], CH[ci][1], CH[ci][0] - NCTX) for ci in range(1, 5)]
        with contextlib.ExitStack() as so:
            wo = [so.enter_context(_sb(nc, "awo%d" % i, [128, KC, 512], BF16)) for i in range(2)]
            wores = [Res(), Res()]
            for half in range(2):
                dma(P, "pool", wo[half][:], Wo.rearrange("(hc p) d -> p hc d", p=128)[:, :, half * 512:(half + 1) * 512], [], [wores[half]])
            for dc in range(KC):
                half, dcl = dc // 4, dc % 4
                for (ci, t0, tn, yc0) in chunk_list:
                    ps, pr = getps(k)
                    for hc in range(KC):
                        mm(P, ps[:, :tn], wo[half][:, hc, dcl * 128:(dcl + 1) * 128], YT[:, hc, yc0:yc0 + tn], hc == 0, hc == KC - 1,
                           [wores[half], yres[hc][ci - 1]], [pr])
                    stt(P, k.hT[:, dc, t0:t0 + tn], ps[:, :tn], k.mod[l][:, 0, 16 + dc:17 + dc], k.hT[:, dc, t0:t0 + tn], ALU.mult, ALU.add,
                        [pr, k.modres, k.hres[dc][ci]], [k.hres[dc][ci]])
            P.barrier()
    k.ps_n = 8


class Chain:
    pass


def scan_chains(k, chains, dk):
    P = k.P
    NCH = T // 64
    pend = {}
    for step in range(NCH + 1):
        cur = {}
        if step < NCH:
            for ci_, ch in enumerate(chains):
                n = ch.order[step]
                t0 = n * 64
                psa, pra = getps(k)
                mm(P, psa[0:64, 0:64], ch.kin[0:dk, t0:t0 + 64], ch.qin[0:dk, t0:t0 + 64], True, True, [ch.kres, ch.qres], [pra])
                psk, prk = getps(k)
                mm(P, psk[0:dk, 0:128], ch.kst[0:64, n, 0:dk], ch.v[0:64, n, :], True, True, [ch.kstres, ch.vres], [prk])
                cur[ci_] = (n, t0, psa, pra, psk, prk)
        outs = {}
        for ci_, ch in enumerate(chains):
            if ci_ in pend:
                m, n, t0 = pend[ci_]
                pso, pro = getps(k)
                mm(P, pso[:, 0:64], ch.v[0:64, n, :], ch.att[m % 2][:, :], True, False, [ch.vres, ch.atres[m % 2]], [pro])
                mm(P, pso[:, 0:64], ch.Sb[(m - 1) % 2][0:dk, :], ch.qin[0:dk, t0:t0 + 64], False, True, [ch.sbres[(m - 1) % 2], ch.qres], [pro])
                outs[ci_] = (n, t0, pso, pro)
        for ci_, ch in enumerate(chains):
            if ci_ in cur:
                n, t0, psa, pra, psk, prk = cur[ci_]
                b = step % 2
                tt(P, "dve", ch.att[b][:, :], psa[0:64, 0:64], ch.mask, ALU.mult, [pra, k.cres], [ch.atres[b]])
            if ci_ in outs:
                n_, t0_, pso, pro = outs[ci_]
                tt(P, "dve", ch.OT[:, t0_:t0_ + 64], pso[:, 0:64], ch.OT[:, t0_:t0_ + 64], ALU.add, [pro, ch.otres[n_]], [ch.otres[n_]])
            if ci_ in cur:
                n, t0, psa, pra, psk, prk = cur[ci_]
                stt(P, ch.S32[0:dk, :], ch.S32[0:dk, :], ch.dec_fn(n), psk[0:dk, 0:128], ALU.mult, ALU.add,
                    [prk, ch.s32res, ch.decres], [ch.s32res])
                act(P, ch.Sb[step % 2][0:dk, :], ch.S32[0:dk, :], AF.Copy, [ch.s32res], [ch.sbres[step % 2]])
        pend = {ci_: (step, cur[ci_][0], cur[ci_][1]) for ci_ in cur}


def make_chain(k, st, name, d, dk, qin, kin, kst, v, OT, otres, qres, kres, kstres, vres, dec_fn, decres, msk):
    nc, P = k.nc, k.P
    ch = Chain()
    ch.order = list(range(T // 64)) if d == 0 else [3, 2, 1, 0] + list(range(T // 64 - 1, 3, -1))
    ch.qin, ch.kin, ch.kst, ch.v, ch.OT, ch.otres = qin, kin, kst, v, OT, otres
    ch.qres, ch.kres, ch.kstres, ch.vres, ch.dec_fn, ch.decres = qres, kres, kstres, vres, dec_fn, decres
    ch.mask = msk[:, d, :]
    ch.att = [st.enter_context(_sb(nc, name + "att%d" % i, [64, 64], BF16)) for i in range(2)]
    ch.atres = [Res() for _ in range(2)]
    ch.S32 = st.enter_context(_sb(nc, name + "S32", [128, 128], F32))
    ch.Sb = [st.enter_context(_sb(nc, name + "Sb%d" % i, [128, 128], BF16)) for i in range(2)]
    ch.s32res, ch.sbres = Res(), [Res(), Res()]
    P.dve(lambda e: e.memset(ch.S32[:], 0.0), [], [ch.s32res])
    P.dve(lambda e: e.memset(ch.Sb[0][:], 0.0), [], [ch.sbres[0]])
    P.dve(lambda e: e.memset(ch.Sb[1][:], 0.0), [], [ch.sbres[1]])
    return ch


def head_finish(k, l, st, name, OT, otres, gT, gres, gain_col, centered, Wo, row0):
    P, nc = k.P, k.nc
    yT = st.enter_context(_sb(nc, name + "yT", [128, T], BF16))
    yres = [Res() for _ in CH]
    sq = st.enter_context(_sb(nc, name + "fsq", [128, 512], F32))
    xc = st.enter_context(_sb(nc, name + "fxc", [128, 512], F32))
    sq2 = st.enter_context(_sb(nc, name + "fsq2", [128, 512], F32))
    xc2 = st.enter_context(_sb(nc, name + "fxc2", [128, 512], F32))
    bufs = [(sq, xc, Res()), (sq2, xc2, Res())]
    keep = {}

    def stage1(ci):
        t0, tn = CH[ci]
        sq_, xc_, R = bufs[ci % 2]
        ors = [otres[n] for n in range(t0 // 64, (t0 + tn) // 64)]
        src = OT[:, t0:t0 + tn]
        if centered:
            psm, prm = getps(k)
            mm(P, psm[:, :tn], k.onesf, src, True, True, ors + [k.cres], [prm])
            stt(P, xc_[:, :tn], psm[:, :tn], -1.0 / 128, src, ALU.mult, ALU.add, [prm] + ors, [R])
            src = xc_[:, :tn]
            ors = [R]
        act(P, sq_[:, :tn], src, AF.Square, ors, [R])
        pss, prs = getps(k)
        mm(P, pss[:, :tn], k.onesf, sq_[:, :tn], True, True, [R, k.cres], [prs])
        rstd_from_ps(k, sq_[:, :tn], pss[:, :tn], prs, R, 1.0 / 128)
        keep[ci] = (src, ors)

    def stage2(ci):
        t0, tn = CH[ci]
        sq_, xc_, R = bufs[ci % 2]
        src, ors = keep[ci]
        stt(P, xc_[:, :tn], src, gain_col, sq_[:, :tn], ALU.mult, ALU.mult, ors + [R, k.cres], [R])
        tt(P, "dve", yT[:, t0:t0 + tn], xc_[:, :tn], gT[:, t0:t0 + tn], ALU.mult, [R, gres], [yres[ci]])

    stage1(0)
    for ci in range(len(CH)):
        if ci + 1 < len(CH):
            stage1(ci + 1)
        stage2(ci)
    chunk_list = [(ci, CH[ci][0], CH[ci][1], CH[ci][0]) for ci in range(len(CH))]
    out_proj_head(k, l, Wo, row0, 128, (lambda yc0, tn: yT[:, yc0:yc0 + tn]), (lambda ci: yres[ci]), chunk_list, name)


def recurrent_mixer(k, l):
    P, nc = k.P, k.nc
    W, Wsw, Wo = k.recw_d, k.recwsw_d, k.recwo_d
    NCH = T // 64
    with contextlib.ExitStack() as st0:
        msk = st0.enter_context(_sb(nc, "rmsk", [64, 2, 64], F32))
        rmask = st0.enter_context(_sb(nc, "rrmask", [128, 512], F32))
        rtab = st0.enter_context(_sb(nc, "rtab_sb", [128, 24, 64], F32))
        rdec = st0.enter_context(_sb(nc, "rdec_sb", [128, 4], F32))
        lbe = st0.enter_context(_sb(nc, "lbe", [128, 8, 3], F32))
        lbs = st0.enter_context(_sb(nc, "lbs", [128, 8], F32))
        lbv = st0.enter_context(_sb(nc, "lbv", [128, 8], F32))
        omlv = st0.enter_context(_sb(nc, "omlv", [128, 8], F32))
        rc = Res()
        dma(P, "sp", msk[:], k.masks_d[:, :, :], [], [rc])
        dma(P, "sp", rtab[:], k.rtab_d[:, :, :], [], [rc])
        dma(P, "sp", rdec[:], k.rdec_d[:, :], [], [rc])
        P.dve(lambda e: e.memset(rmask[:], 1.0), [], [rc])
        P.dve(lambda e: e.memset(rmask[:, :].rearrange("p (n c) -> p n c", c=64)[:, :, 0:1], 0.0), [rc], [rc])
        act(P, lbe[:], k.vecs[:, V_REC:V_REC + 24].rearrange("p (a s) -> p a s", s=3), AF.Exp, [k.cres], [rc])
        P.dve(lambda e: e.reduce_sum(out=lbs[:], in_=lbe[:], axis=AX.X), [rc], [rc])
        P.dve(lambda e: e.reciprocal(out=lbs[:], in_=lbs[:]), [rc], [rc])
        P.dve(lambda e: e.reduce_sum(out=lbv[:], in_=lbe[:, :, 0:l + 1], axis=AX.X), [rc], [rc])
        tt(P, "dve", lbv[:], lbv[:], lbs[:], ALU.mult, [rc], [rc])
        ts(P, "dve", omlv[:], lbv[:], -1.0, 1.0, ALU.mult, ALU.add, [rc], [rc])

        for hh in range(4):
            with contextlib.ExitStack() as st:
                nm = "hg%d" % hh
                qin = [st.enter_context(_sb(nc, nm + "qin%d" % d, [128, T], BF16)) for d in range(2)]
                kin = [st.enter_context(_sb(nc, nm + "kin%d" % d, [128, T], BF16)) for d in range(2)]
                kst = [st.enter_context(_sb(nc, nm + "kst%d" % d, [64, NCH, 128], BF16)) for d in range(2)]
                dec = [st.enter_context(_sb(nc, nm + "dec%d" % d, [128, NCH], F32)) for d in range(2)]
                v = st.enter_context(_sb(nc, nm + "v", [64, NCH, 128], BF16))
                gT = st.enter_context(_sb(nc, nm + "gT", [128, T], BF16))
                qres, kres, kstres, decres = [Res(), Res()], [Res(), Res()], [Res(), Res()], [Res(), Res()]
                vres, gres = Res(), Res()
                v_prep(k, nm + "v", W, 1536 + hh * 128, 128, v, vres, rows=64)
                with contextlib.ExitStack() as s2:
                    wt = s2.enter_context(_sb(nc, nm + "w", [128, KC, 4, 128], BF16))
                    wres = Res()
                    for i, c0 in enumerate((hh * 128, 512 + hh * 128, 1024 + hh * 128, 2048 + hh * 128)):
                        wload(k, wt[:, :, i, :], W, c0, 128, wres)
                    tmp = [s2.enter_context(_sb(nc, nm + "tmp%d" % i, [128, 512], F32)) for i in range(8)]
                    qf, fg, lf, kk, bs, b2, ee, kT = tmp
                    R = Res()
                    for ci, (t0, tn) in enumerate(CH):
                        n0, nn = t0 // 64, tn // 64
                        psq, prq = getps(k)
                        fm_proj(k, psq[:, :tn], prq, wt[:, :, 0, :], wres, 0, 128, ci, t0, tn)
                        act(P, qf[:, :tn], psq[:, :tn], AF.Silu, [prq], [R])
                        psg, prg = getps(k)
                        fm_proj(k, psg[:, :tn], prg, wt[:, :, 3, :], wres, 0, 128, ci, t0, tn)
                        act(P, gT[:, t0:t0 + tn], psg[:, :tn], AF.Silu, [prg], [gres])
                        for d in range(2):
                            j = d * 4 + hh
                            psf, prf = getps(k)
                            fm_proj(k, psf[:, :tn], prf, wt[:, :, 1 + d, :], wres, 0, 128, ci, t0, tn)
                            act(P, fg[:, :tn], psf[:, :tn], AF.Sigmoid, [prf], [R])
                            ts(P, "dve", fg[:, :tn], fg[:, :tn], omlv[:, j:j + 1], lbv[:, j:j + 1], ALU.mult, ALU.add, [R, rc], [R])
                            act(P, lf[:, :tn], fg[:, :tn], AF.Ln, [R], [R])
                            ts(P, "dve", kk[:, :tn], fg[:, :tn], -1.0, 1.0, ALU.mult, ALU.add, [R], [R])
                            P.dve(lambda e, o=bs[:, :tn], m=rmask[:, :tn], x=lf[:, :tn]: e.tensor_tensor_scan(
                                out=o, data0=m, data1=x, initial=0.0, op0=ALU.mult, op1=ALU.add), [R, rc], [R])
                            bs3 = bs[:, :tn].rearrange("p (n c) -> p n c", c=64)
                            totb = bs3[:, :, 63:64].broadcast_to([128, nn, 64])
                            act(P, dec[d][:, n0:n0 + nn], bs3[:, :, 63], AF.Exp, [R], [decres[d]])
                            v3 = lambda t: t[:, :tn].rearrange("p (n c) -> p n c", c=64)
                            tt(P, "dve", v3(b2), totb, bs3, ALU.subtract, [R], [R])
                            if d == 0:
                                bcur = bs
                                act(P, ee[:, :tn], b2[:, :tn], AF.Exp, [R], [R])
                            else:
                                tt(P, "dve", ee[:, :tn], bs[:, :tn], lf[:, :tn], ALU.subtract, [R], [R])
                                act(P, ee[:, :tn], ee[:, :tn], AF.Exp, [R], [R])
                                tt(P, "dve", b2[:, :tn], b2[:, :tn], lf[:, :tn], ALU.add, [R], [R])
                                bcur = b2
                            tt(P, "dve", kT[:, :tn], kk[:, :tn], ee[:, :tn], ALU.mult, [R], [R])
                            for g4 in range(0, nn, 4):
                                pst, prt = getps(k)
                                m4 = min(4, nn - g4)
                                for jj in range(m4):
                                    tr(P, pst[0:64, jj * 128:(jj + 1) * 128], kT[:, (g4 + jj) * 64:(g4 + jj + 1) * 64], k.identf, [R, k.cres], [prt])
                                cp(P, "dve", kst[d][0:64, n0 + g4:n0 + g4 + m4, :], pst[0:64, 0:m4 * 128].rearrange("p (a f) -> p a f", f=128),
                                   [prt], [kstres[d]])
                            act(P, ee[:, :tn], bcur[:, :tn], AF.Exp, [R], [R])
                            stt(P, qin[d][:, t0:t0 + tn], qf[:, :tn], 128 ** -0.5, ee[:, :tn], ALU.mult, ALU.mult, [R], [qres[d]])
                            act(P, ee[:, :tn], bcur[:, :tn], AF.Exp, [R], [R], scale=-1.0)
                            tt(P, "dve", kin[d][:, t0:t0 + tn], kk[:, :tn], ee[:, :tn], ALU.mult, [R], [kres[d]])
                    P.barrier()
                OT = st.enter_context(_sb(nc, nm + "OT", [128, T], F32))
                otres = [Res() for _ in range(NCH)]
                P.pool(lambda e, o=OT: e.memset(o[:], 0.0), [], otres)
                chains = [make_chain(k, st, nm + "c%d" % d, d, 128, qin[d], kin[d], kst[d], v, OT, otres, qres[d], kres[d], kstres[d], vres,
                                     (lambda n, d=d: dec[d][:, n:n + 1]), decres[d], msk) for d in range(2)]
                scan_chains(k, chains, 128)
                head_finish(k, l, st, nm, OT, otres, gT, gres, k.vecs[:, V_REC + 24:V_REC + 25], False, Wo, hh * 128)
                P.barrier()

        for r in range(4):
            with contextlib.ExitStack() as st:
                nm = "rt%d" % r
                qin = [st.enter_context(_sb(nc, nm + "qin%d" % d, [64, T], BF16)) for d in range(2)]
                kin = [st.enter_context(_sb(nc, nm + "kin%d" % d, [64, T], BF16)) for d in range(2)]
                kst = [st.enter_context(_sb(nc, nm + "kst%d" % d, [64, NCH, 64], BF16)) for d in range(2)]
                v = st.enter_context(_sb(nc, nm + "v", [64, NCH, 128], BF16))
                gT = st.enter_context(_sb(nc, nm + "gT", [128, T], BF16))
                qres, kres, kstres = [Res(), Res()], [Res(), Res()], [Res(), Res()]
                vres, gres = Res(), Res()
                v_prep(k, nm + "v", W, 3072 + r * 128, 128, v, vres, rows=64)
                with contextlib.ExitStack() as s2:
                    wg = s2.enter_context(_sb(nc, nm + "wg", [128, KC, 128], BF16))
                    wres = Res()
                    wload(k, wg[:], W, 3584 + r * 128, 128, wres)
                    for ci, (t0, tn) in enumerate(CH):
                        psg, prg = getps(k)
                        fm_proj(k, psg[:, :tn], prg, wg, wres, 0, 128, ci, t0, tn)
                        act(P, gT[:, t0:t0 + tn], psg[:, :tn], AF.Silu, [prg], [gres])
                    qf = s2.enter_context(_sb(nc, nm + "qf", [64, T], F32))
                    kf = s2.enter_context(_sb(nc, nm + "kf", [64, T], F32))
                    qfr = [Res() for _ in CH]
                    kfr = [Res() for _ in CH]
                    qk_prep(k, nm + "q", W, Wsw, 2560 + r * 64, 64, None, None, None, 64, 4, qf, (lambda ci: qfr[ci]), True, post_scale=1.0, norm=False, dbuf=False)
                    qk_prep(k, nm + "k", W, Wsw, 2816 + r * 64, 64, None, None, None, 64, 4, kf, (lambda ci: kfr[ci]), True, post_scale=64 ** -0.5, norm=False, dbuf=False)
                    kT = s2.enter_context(_sb(nc, nm + "kT", [64, 512], F32))
                    R = Res()
                    for d in range(2):
                        gi = r if d == 0 else 3 - r
                        tb = lambda kind, nn: rtab[0:64, (gi * 2 + d) * 3 + kind:(gi * 2 + d) * 3 + kind + 1, :].broadcast_to([64, nn, 64])
                        for ci, (t0, tn) in enumerate(CH):
                            n0, nn = t0 // 64, tn // 64
                            v3 = lambda ap: ap.rearrange("p (n c) -> p n c", c=64)
                            tt(P, "dve", v3(qin[d][:, t0:t0 + tn]), v3(qf[:, t0:t0 + tn]), tb(0, nn), ALU.mult, [qfr[ci], rc], [qres[d]])
                            tt(P, "dve", v3(kin[d][:, t0:t0 + tn]), v3(kf[:, t0:t0 + tn]), tb(1, nn), ALU.mult, [kfr[ci], rc], [kres[d]])
                            tt(P, "dve", v3(kT[:, :tn]), v3(kf[:, t0:t0 + tn]), tb(2, nn), ALU.mult, [kfr[ci], rc, R], [R])
                            pst, prt = getps(k)
                            for jj in range(nn):
                                tr(P, pst[0:64, jj * 64:(jj + 1) * 64], kT[:, jj * 64:(jj + 1) * 64], k.identf[0:64, 0:64], [R, k.cres], [prt])
                            cp(P, "dve", kst[d][0:64, n0:n0 + nn, :], pst[0:64, 0:nn * 64].rearrange("p (a f) -> p a f", f=64), [prt], [kstres[d]])
                    P.barrier()
                OT = st.enter_context(_sb(nc, nm + "OT", [128, T], F32))
                otres = [Res() for _ in range(NCH)]
                P.pool(lambda e, o=OT: e.memset(o[:], 0.0), [], otres)
                chains = []
                for d in range(2):
                    gi = r if d == 0 else 3 - r
                    chains.append(make_chain(k, st, nm + "c%d" % d, d, 64, qin[d], kin[d], kst[d], v, OT, otres, qres[d], kres[d], kstres[d], vres,
                                             (lambda n, gi=gi: rdec[0:64, gi:gi + 1]), rc, msk))
                scan_chains(k, chains, 64)
                head_finish(k, l, st, nm, OT, otres, gT, gres, k.vecs[:, V_REC + 25:V_REC + 26], True, Wo, 512 + r * 128)
                P.barrier()


def build(cfg):
    nc = bass.Bass("TRN2", target_bir_lowering=False)
    k = K()
    k.nc = nc
    k.P = P = Prog()

    def din(name, shape):
        return nc.dram_tensor(name, list(shape), F32, kind="ExternalInput").ap()

    k.x_d = din("x", [NLAT, D])
    k.ctx_d = din("ctx", [NCTX, D])
    k.cc_d = din("cc", [128, KC, 2])
    k.vecs_d = din("vecs", [128, NVEC])
    k.wada_d = din("w_ada", [DEPTH, D, 6 * D])
    k.wrouter_d = din("w_router", [DEPTH, D, NE])
    k.brouter_d = din("b_router", [DEPTH, NE])
    k.wgu_d = din("w_gu", [DEPTH, NE, D, 2 * D])
    k.wdown_d = din("w_down", [DEPTH, NE, D, D])
    k.bdown_d = din("b_down", [DEPTH, NE, D])
    k.consts_d = din("consts", [128, 3 * 128])
    k.attw_d = din("att_w", [D, 2560])
    k.attwsw_d = din("att_w_sw", [D, 2560])
    k.attwo_d = din("att_wo", [D, D])
    k.recw_d = din("rec_w", [D, 4096])
    k.recwsw_d = din("rec_w_sw", [D, 4096])
    k.recwo_d = din("rec_wo", [D, D])
    k.rope_d = din("rope", [6, 128, NLAT])
    k.dlam_d = din("dlam", [128, 4, 64])
    k.masks_d = din("masks", [64, 2, 64])
    k.rtab_d = din("rtab", [128, 24, 64])
    k.rdec_d = din("rdec", [128, 4])
    k.out_d = nc.dram_tensor("out", [NLAT, D], F32, kind="ExternalOutput").ap()
    dbg = {}
    for name, shape in cfg.get("debug", {}).items():
        dbg[name] = nc.dram_tensor(name, list(shape), F32, kind="ExternalOutput").ap()
    k.dbg = dbg
    k.dbg_ops = []

    with contextlib.ExitStack() as st:
        k.hT = st.enter_context(_sb(nc, "hT", [128, KC, T], F32))
        k.hres = [[Res() for _ in CH] for _ in range(KC)]
        k.xT = st.enter_context(_sb(nc, "xT", [128, KC, T], BF16))
        k.xres = [[Res() for _ in CH] for _ in range(KC)]
        k.vecs = st.enter_context(_sb(nc, "vecs_sb", [128, NVEC], F32))
        cst = st.enter_context(_sb(nc, "consts_sb", [128, 3 * 128], F32))
        k.identf = cst[:, 0:128]
        k.onesf = cst[:, 128:256]
        k.ones2 = cst[:, 256:384]
        k.epsc = st.enter_context(_sb(nc, "epsc", [128, 1], F32))
        k.cres = Res("consts")
        k.mod = [st.enter_context(_sb(nc, "mod%d" % l, [128, 2, 48], F32)) for l in range(DEPTH)]
        k.A1 = [st.enter_context(_sb(nc, "A1_%d" % l, [128, 2, KC], F32)) for l in range(DEPTH)]
        k.A2 = [st.enter_context(_sb(nc, "A2_%d" % l, [128, 2, KC], F32)) for l in range(DEPTH)]
        k.modres = Res("mod")
        k.ps = [st.enter_context(nc.psum_tensor("ps%d" % i, [128, 512], F32)) for i in range(8)]
        k.psr = [Res() for _ in range(8)]
        k.ps_next = 0
        k.ps_n = 8

        dma(P, "sp", k.vecs[:], k.vecs_d[:, :], [], [k.cres])
        dma(P, "sp", cst[:], k.consts_d[:, :], [], [k.cres])
        P.pool(lambda e: e.memset(k.epsc[:], EPS), [], [k.cres])

        with contextlib.ExitStack() as st_ld:
            load_tokens(k, st_ld)
            modulation(k)
        final_ops = []
        for l in range(DEPTH):
            if l not in cfg.get("layer_list", range(DEPTH)):
                continue
            if "mixer" in cfg.get("stages", ()):
                norm_mod(k, l, 0)
                if l % 2 == 0:
                    recurrent_mixer(k, l)
                else:
                    attention_mixer(k, l)
            if "moe" in cfg.get("stages", ("moe",)):
                moe_layer(k, l)
            if cfg.get("layers", DEPTH) == l + 1:
                break
        for name, (kind, arg) in cfg.get("dump", {}).items():
            pass
        final_ops += store_output(k)
        final_ops += k.dbg_ops
        P.emit(nc, final_wait_ops=final_ops)
    return nc


def host_inputs(inputs, b):
    f = lambda a: np.ascontiguousarray(np.asarray(a, dtype=np.float32))
    cc = np.stack([np.asarray(inputs["c"])[b], np.asarray(inputs["c_ctx"])], axis=-1)
    cc = cc.reshape(KC, 128, 2).transpose(1, 0, 2)
    vecs = np.zeros((128, NVEC), np.float32)

    def fm(v):
        v = np.asarray(v, np.float32)
        return v.reshape(-1, 128).T

    for l in range(DEPTH):
        vecs[:, V_BADA + l * 48:V_BADA + (l + 1) * 48] = fm(inputs["b_ada"][l])
        vecs[:, V_NMIX + l * 8:V_NMIX + (l + 1) * 8] = fm(inputs["norm_mix"][l])
        vecs[:, V_NFFN + l * 8:V_NFFN + (l + 1) * 8] = fm(inputs["norm_ffn"][l])
        for e in range(NE):
            o = V_BGU + (l * NE + e) * 16
            vecs[:, o:o + 16] = fm(inputs["b_gu"][l, e])
    sw = _pairswap(128)
    g = lambda n: np.asarray(inputs[n][0], np.float32)
    qg, kg = g("att_q_gain"), g("att_k_gain")
    dq2, dk2 = np.concatenate([g("diff_q_gain")] * 2), np.concatenate([g("diff_k_gain")] * 2)
    for i, vcol in enumerate((qg, qg[sw], kg, kg[sw], dq2, dq2[sw], dk2, dk2[sw], g("diff_gain"))):
        vecs[:, V_ATT + i] = vcol
    lbl = np.asarray(inputs["rec_lb_logits"], np.float32)
    for d in range(2):
        for hh in range(4):
            for sl in range(3):
                vecs[:, V_REC + (d * 4 + hh) * 3 + sl] = lbl[sl, d, hh * 128:(hh + 1) * 128]
    vecs[:, V_REC + 24] = g("rec_hg_gain")
    vecs[:, V_REC + 25] = g("rec_ret_gain")
    consts = np.zeros((128, 3 * 128), np.float32)
    consts[:, 0:128] = np.eye(128, dtype=np.float32)
    consts[:, 128:256] = 1.0
    consts[0:64, 256:320] = 1.0
    consts[64:128, 320:384] = 1.0
    out = {
        "x": f(inputs["x"][b]), "ctx": f(inputs["ctx"][b]), "cc": f(cc), "vecs": vecs,
        "w_ada": f(inputs["w_ada"]), "w_router": f(inputs["w_router"]), "b_router": f(inputs["b_router"]),
        "w_gu": f(inputs["w_gu"]), "w_down": f(inputs["w_down"]), "b_down": f(inputs["b_down"]),
        "consts": consts,
    }
    out.update(_shared_host(inputs))
    return out


def _pairswap(n):
    p = np.arange(n)
    return p + 1 - 2 * (p % 2)


_SHARED = {}


def _shared_host(inputs):
    key = id(inputs["w_gu"])
    if _SHARED.get("key") == key:
        return _SHARED["val"]
    f = lambda a: np.ascontiguousarray(np.asarray(a, dtype=np.float32))
    aw = np.asarray(inputs["att_w_in"][0], np.float32)
    rw = np.asarray(inputs["rec_w_in"][0], np.float32)
    val = {
        "att_w": f(aw), "att_w_sw": f(aw[:, _pairswap(aw.shape[1])]), "att_wo": f(inputs["att_w_out"][0]),
        "rec_w": f(rw), "rec_w_sw": f(rw[:, _pairswap(rw.shape[1])]), "rec_wo": f(inputs["rec_w_out"][0]),
        "dlam": f(np.broadcast_to(np.asarray(inputs["diff_lambda"][0], np.float32)[None], (128, 4, 64))),
    }
    rope = np.zeros((6, 128, NLAT), np.float32)
    t = np.arange(NLAT)
    row = (t // 64).astype(np.float32)
    colp = (t % 64).astype(np.float32)
    for idx, hd in ((0, 128), (2, 64), (4, 64)):
        axis_dim = hd // 2
        inv_freq = (np.float32(10000.0) ** (-np.arange(0, axis_dim, 2, dtype=np.float32) / np.float32(axis_dim))).astype(np.float32)
        ang = np.concatenate([row[:, None] * inv_freq[None, :], colp[:, None] * inv_freq[None, :]], axis=-1).astype(np.float32)
        c = np.cos(ang).astype(np.float32)
        sn = np.sin(ang).astype(np.float32)
        C = np.repeat(c, 2, axis=1).T
        S = np.repeat(sn, 2, axis=1).T
        S[0::2, :] *= -1.0
        reps = 128 // hd
        rope[idx] = np.tile(C, (reps, 1))
        rope[idx + 1] = np.tile(S, (reps, 1))
    val["rope"] = rope
    s_i = np.arange(64)[:, None]
    c_i = np.arange(64)[None, :]
    masks = np.stack([(s_i <= c_i), (s_i >= c_i)], axis=1).astype(np.float32)
    val["masks"] = f(masks)
    rtab = np.zeros((128, 24, 64), np.float32)
    rdec = np.zeros((128, 4), np.float32)
    p = np.arange(64, dtype=np.float64)
    for gi in range(4):
        lg = np.log(np.float64(np.float32(1.0) - np.float32(2.0) ** np.float32(-5.0 - gi)))
        rdec[:, gi] = np.exp(64 * lg)
        for d in range(2):
            bb = (p + 1) * lg if d == 0 else (64 - p) * lg
            o = (gi * 2 + d) * 3
            rtab[:, o + 0, :] = np.exp(bb)[None, :]
            rtab[:, o + 1, :] = np.exp(-bb)[None, :]
            rtab[:, o + 2, :] = np.exp(64 * lg - bb)[None, :]
    val["rtab"] = rtab
    val["rdec"] = rdec
    _SHARED["key"] = key
    _SHARED["val"] = val
    return val


_CACHE = {}


def kernel(**inputs):
    cfg = {"stages": ("mixer", "moe")}
    if "nc" not in _CACHE:
        _CACHE["nc"] = build(cfg)
    nc = _CACHE["nc"]
    n = 8
    in_maps = [host_inputs(inputs, b) for b in range(n)]
    res = run_bass_kernel_spmd(nc, in_maps, core_ids=list(range(n)))
    return np.stack([r["out"] for r in res.results], axis=0).astype(np.float32)
```
